# Optimizing a Trainium2 kernel written in Bass

```python
import math
import jax
import jax.numpy as jnp
from jax import lax
import numpy as np

D_MODEL = 1024
BATCH = 16
SEQ = 2048
DEPTH = 1

RET_HEADS = 4
RET_DK = 128
RET_DV = D_MODEL // RET_HEADS
GDN_HEADS = 4
GDN_DK = 128
GDN_DV = D_MODEL // GDN_HEADS
CHUNK = 64
CONV_K = 4
N_GROUPS = 4
EXPERTS_PER_GROUP = 4
TOP_K = 2
D_FF_EXPERT = 512
ROPE_BASE = 10000.0
NORM_EPS = 1e-6
L2_EPS = 1e-6
N_MOD = 6

RET_QK = RET_HEADS * RET_DK
RET_V = RET_HEADS * RET_DV
GDN_QK = GDN_HEADS * GDN_DK
GDN_V = GDN_HEADS * GDN_DV
IN_WIDTHS = (RET_QK, RET_QK, RET_V, RET_V, GDN_QK, GDN_QK, GDN_V, GDN_V, GDN_HEADS, GDN_HEADS, D_MODEL, D_MODEL)
D_IN = sum(IN_WIDTHS)
F32 = jnp.float32

kernel_name = 'hybrid_retention_gdn_hmoe_block'


def _rms_norm(x, w=None):
    xf = x.astype(F32)
    y = xf * lax.rsqrt(jnp.mean(xf * xf, axis=-1, keepdims=True) + NORM_EPS)
    if w is not None:
        y = y * w.astype(F32)
    return y.astype(x.dtype)


def _l2_normalize(x):
    xf = x.astype(F32)
    return xf * lax.rsqrt(jnp.sum(xf * xf, axis=-1, keepdims=True) + L2_EPS)


def _split_columns(t, widths):
    out, start = [], 0
    for w in widths:
        out.append(t[..., start:start + w])
        start += w
    return out


def _rope(x):
    d = x.shape[-1]
    half = d // 2
    inv_freq = 1.0 / (ROPE_BASE ** (jnp.arange(half, dtype=F32) / half))
    ang = jnp.arange(x.shape[1], dtype=F32)[:, None] * inv_freq[None, :]
    cos = jnp.cos(ang)[None, :, None, :]
    sin = jnp.sin(ang)[None, :, None, :]
    xf = x.astype(F32)
    x1, x2 = xf[..., :half], xf[..., half:]
    return jnp.concatenate([x1 * cos - x2 * sin, x1 * sin + x2 * cos], axis=-1)


def _to_chunks(t):
    b, s, h, d = t.shape
    return t.reshape(b, s // CHUNK, CHUNK, h, d).transpose(0, 3, 1, 2, 4)


def _from_chunks(t):
    b, h, n, c, d = t.shape
    return t.transpose(0, 2, 3, 1, 4).reshape(b, n * c, h, d)


def _causal_conv(x, w):
    s = x.shape[1]
    xp = jnp.pad(x, ((0, 0), (CONV_K - 1, 0), (0, 0)))
    out = xp[:, 0:s] * w[0]
    for i in range(1, CONV_K):
        out = out + xp[:, i:i + s] * w[i]
    return out


def _retention_chunkwise(q, k, v):
    n_heads, dk = q.shape[2], q.shape[3]
    q = _to_chunks(q.astype(F32))
    k = _to_chunks(k.astype(F32)) * (dk ** -0.5)
    v = _to_chunks(v.astype(F32))
    b, h, _, _, dv = v.shape
    log_gamma = jnp.log(1.0 - 2.0 ** (-5.0 - jnp.arange(n_heads, dtype=F32)))
    pos = jnp.arange(CHUNK, dtype=F32)
    rel = pos[:, None] - pos[None, :]
    causal = rel >= 0
    intra_decay = jnp.where(causal, jnp.exp(jnp.where(causal, rel, 0.0) * log_gamma[:, None, None]), 0.0)
    q_decay = jnp.exp((pos + 1.0) * log_gamma[:, None])
    k_decay = jnp.exp((CHUNK - 1.0 - pos) * log_gamma[:, None])
    chunk_decay = jnp.exp(CHUNK * log_gamma)[None, :, None, None]
    scores = jnp.einsum('bhnid,bhnjd->bhnij', q, k) * intra_decay[None, :, None]
    o_intra = jnp.einsum('bhnij,bhnjv->bhniv', scores, v)
    q_in = q * q_decay[None, :, None, :, None]
    k_in = k * k_decay[None, :, None, :, None]

    def step(state, xs):
        q_n, k_n, v_n = xs
        o_n = jnp.einsum('bhcd,bhdv->bhcv', q_n, state)
        state = state * chunk_decay + jnp.einsum('bhcd,bhcv->bhdv', k_n, v_n)
        return state, o_n

    state0 = jnp.zeros((b, h, dk, dv), F32)
    _, o_inter = lax.scan(step, state0, (jnp.moveaxis(q_in, 2, 0), jnp.moveaxis(k_in, 2, 0), jnp.moveaxis(v, 2, 0)))
    return _from_chunks(o_intra + jnp.moveaxis(o_inter, 0, 2))


def _gated_delta_rule_chunked(q, k, v, log_decay, beta):
    dk = q.shape[-1]
    q = _to_chunks(q.astype(F32)) * (dk ** -0.5)
    k = _to_chunks(k.astype(F32))
    v = _to_chunks(v.astype(F32))
    b, h, n, c, dv = v.shape
    g = log_decay.astype(F32).reshape(b, n, c, h).transpose(0, 3, 1, 2)
    bt = beta.astype(F32).reshape(b, n, c, h).transpose(0, 3, 1, 2)
    g_cum = jnp.cumsum(g, axis=-1)
    idx = jnp.arange(CHUNK)
    causal = idx[:, None] >= idx[None, :]
    strict = idx[:, None] > idx[None, :]
    decay = jnp.exp(jnp.where(causal, g_cum[..., :, None] - g_cum[..., None, :], -jnp.inf))
    k_beta = k * bt[..., None]
    v_beta = v * bt[..., None]
    a_mat = jnp.where(strict, jnp.einsum('bhnid,bhnjd->bhnij', k_beta, k) * decay, 0.0) + jnp.eye(CHUNK, dtype=F32)
    rhs = jnp.concatenate([v_beta, k_beta * jnp.exp(g_cum)[..., None]], axis=-1)
    sol = lax.linalg.triangular_solve(a_mat, rhs, left_side=True, lower=True, unit_diagonal=True)
    u, w = sol[..., :dv], sol[..., dv:]
    qk = jnp.einsum('bhnid,bhnjd->bhnij', q, k) * decay
    q_g = q * jnp.exp(g_cum)[..., None]
    g_last = g_cum[..., -1]
    k_g = k * jnp.exp(g_last[..., None] - g_cum)[..., None]

    def step(state, xs):
        u_n, w_n, qk_n, q_n, k_n, gl_n = xs
        v_new = u_n - jnp.einsum('bhcd,bhdv->bhcv', w_n, state)
        o_n = jnp.einsum('bhcd,bhdv->bhcv', q_n, state) + jnp.einsum('bhij,bhjv->bhiv', qk_n, v_new)
        state = state * jnp.exp(gl_n)[..., None, None] + jnp.einsum('bhcd,bhcv->bhdv', k_n, v_new)
        return state, o_n

    xs = tuple(jnp.moveaxis(t, 2, 0) for t in (u, w, qk, q_g, k_g, g_last))
    state0 = jnp.zeros((b, h, dk, dv), F32)
    _, o = lax.scan(step, state0, xs)
    return _from_chunks(jnp.moveaxis(o, 0, 2))


def _hybrid_mixer(xm, w_in, conv_w, a_log, dt_bias, gdn_norm_w, w_out):
    b, s, _ = xm.shape
    proj = jnp.einsum('bsd,de->bse', xm, w_in)
    rq, rk, rv, rg, gq, gk, gv, gz, ga, gb, gate_a, gate_b = _split_columns(proj, IN_WIDTHS)
    q_r = _rope(rq.reshape(b, s, RET_HEADS, RET_DK))
    k_r = _rope(rk.reshape(b, s, RET_HEADS, RET_DK))
    o_r = _retention_chunkwise(q_r, k_r, rv.reshape(b, s, RET_HEADS, RET_DV))
    o_r = _rms_norm(o_r).reshape(b, s, RET_V).astype(xm.dtype)
    y_ret = jax.nn.silu(rg) * o_r
    qkv = jax.nn.silu(_causal_conv(jnp.concatenate([gq, gk, gv], axis=-1), conv_w))
    q_g, k_g, v_g = _split_columns(qkv, (GDN_QK, GDN_QK, GDN_V))
    q_g = _l2_normalize(q_g.reshape(b, s, GDN_HEADS, GDN_DK))
    k_g = _l2_normalize(k_g.reshape(b, s, GDN_HEADS, GDN_DK))
    beta = jax.nn.sigmoid(gb.astype(F32))
    log_decay = -jnp.exp(a_log.astype(F32)) * jax.nn.softplus(ga.astype(F32) + dt_bias.astype(F32))
    o_g = _gated_delta_rule_chunked(q_g, k_g, v_g.reshape(b, s, GDN_HEADS, GDN_DV), log_decay, beta)
    o_g = _rms_norm(o_g, gdn_norm_w) * jax.nn.silu(gz.reshape(b, s, GDN_HEADS, GDN_DV).astype(F32))
    y_gdn = o_g.reshape(b, s, GDN_V).astype(xm.dtype)
    merged = jax.nn.sigmoid(gate_a) * y_ret + jax.nn.sigmoid(gate_b) * y_gdn
    return jnp.einsum('bsd,de->bse', merged, w_out)


def _hierarchical_moe(xm, w_group, b_group, w_router, b_router, w_gate, w_up, w_down):
    b, s, d = xm.shape
    xf = xm.reshape(-1, d)
    t = xf.shape[0]
    group_prob = jax.nn.softmax((xf @ w_group).astype(F32) + b_group.astype(F32), axis=-1)
    g_top, g_idx = lax.top_k(group_prob, 1)
    exp_logits = (xf @ w_router).astype(F32).reshape(t, N_GROUPS, EXPERTS_PER_GROUP) + b_router.astype(F32)
    exp_logits = jnp.take_along_axis(exp_logits, g_idx[:, :, None], axis=1)[:, 0]
    exp_prob = jax.nn.softmax(exp_logits, axis=-1)
    e_top, e_idx = lax.top_k(exp_prob, TOP_K)
    e_w = e_top / jnp.sum(e_top, axis=-1, keepdims=True)
    within = jnp.sum(jax.nn.one_hot(e_idx, EXPERTS_PER_GROUP, dtype=F32) * e_w[..., None], axis=1)
    comb = jax.nn.one_hot(g_idx[:, 0], N_GROUPS, dtype=F32)[:, :, None] * (g_top[:, :, None] * within[:, None, :])
    comb = comb.astype(xm.dtype)
    out = jnp.zeros_like(xf)
    for gi in range(N_GROUPS):
        hg = jnp.einsum('td,edf->tef', xf, w_gate[gi])
        hu = jnp.einsum('td,edf->tef', xf, w_up[gi])
        act = jax.nn.silu(hg) * hu * comb[:, gi, :, None]
        out = out + jnp.einsum('tef,efd->td', act, w_down[gi])
    return out.reshape(b, s, d)


def setup_inputs(seed: int = 0) -> dict:
    key = jax.random.key(seed)
    ks = jax.random.split(key, 20)
    L, G, E, F = DEPTH, N_GROUPS, EXPERTS_PER_GROUP, D_FF_EXPERT

    def nrm(k, shape, scale):
        return jax.random.normal(k, shape, F32) * scale

    dt = jnp.exp(jax.random.uniform(ks[8], (L, GDN_HEADS), F32, math.log(1e-3), math.log(1e-1)))
    return {
        'x': nrm(ks[0], (BATCH, SEQ, D_MODEL), 1.0),
        'c': nrm(ks[1], (BATCH, D_MODEL), 1.0),
        'mod_w': nrm(ks[2], (L, D_MODEL, N_MOD * D_MODEL), 0.5 * D_MODEL ** -0.5),
        'mod_b': nrm(ks[3], (L, N_MOD * D_MODEL), 0.02),
        'norm_mix_w': 1.0 + nrm(ks[4], (L, D_MODEL), 0.02),
        'w_in': nrm(ks[5], (L, D_MODEL, D_IN), D_MODEL ** -0.5),
        'gdn_conv_w': nrm(ks[6], (L, CONV_K, 2 * GDN_QK + GDN_V), CONV_K ** -0.5),
        'gdn_a_log': jnp.log(jax.random.uniform(ks[7], (L, GDN_HEADS), F32, 1.0, 16.0)),
        'gdn_dt_bias': dt + jnp.log(-jnp.expm1(-dt)),
        'gdn_norm_w': 1.0 + nrm(ks[9], (L, GDN_DV), 0.02),
        'w_out': nrm(ks[10], (L, D_MODEL, D_MODEL), D_MODEL ** -0.5),
        'norm_ffn_w': 1.0 + nrm(ks[11], (L, D_MODEL), 0.02),
        'w_group': nrm(ks[12], (L, D_MODEL, G), D_MODEL ** -0.5),
        'b_group': nrm(ks[13], (L, G), 0.01),
        'w_router': nrm(ks[14], (L, D_MODEL, G * E), D_MODEL ** -0.5),
        'b_router': nrm(ks[15], (L, G, E), 0.01),
        'w_gate': nrm(ks[16], (L, G, E, D_MODEL, F), D_MODEL ** -0.5),
        'w_up': nrm(ks[17], (L, G, E, D_MODEL, F), D_MODEL ** -0.5),
        'w_down': nrm(ks[18], (L, G, E, F, D_MODEL), F ** -0.5),
        'norm_out_w': 1.0 + nrm(ks[19], (D_MODEL,), 0.02),
    }


def reference(x, c, mod_w, mod_b, norm_mix_w, w_in, gdn_conv_w, gdn_a_log, gdn_dt_bias, gdn_norm_w, w_out,
              norm_ffn_w, w_group, b_group, w_router, b_router, w_gate, w_up, w_down, norm_out_w):
    h = x
    for l in range(DEPTH):
        mod = jax.nn.silu(c) @ mod_w[l] + mod_b[l]
        shift_m, scale_m, gate_m, shift_f, scale_f, gate_f = jnp.split(mod[:, None, :], N_MOD, axis=-1)
        xm = _rms_norm(h, norm_mix_w[l]) * (1.0 + scale_m) + shift_m
        h = h + gate_m * _hybrid_mixer(xm, w_in[l], gdn_conv_w[l], gdn_a_log[l], gdn_dt_bias[l], gdn_norm_w[l], w_out[l])
        xf = _rms_norm(h, norm_ffn_w[l]) * (1.0 + scale_f) + shift_f
        h = h + gate_f * _hierarchical_moe(xf, w_group[l], b_group[l], w_router[l], b_router[l], w_gate[l], w_up[l], w_down[l])
    return _rms_norm(h, norm_out_w)
```

```python
import threading
import numpy as np
import concourse.bass as bass
import concourse.mybir as mybir
from concourse.bass_utils import run_bass_kernel_spmd

F32 = mybir.dt.float32
BF16 = mybir.dt.bfloat16
AF = mybir.ActivationFunctionType
ALU = mybir.AluOpType
AX = mybir.AxisListType

SEQ = 2048
TOK = 1024
NBLK = TOK // 512
DM = 1024
NCORES = 8
SPC = 2
WCOLS = 1280
DK_SCALE = 128.0 ** -0.5

O_RQ, O_RK, O_RV, O_RG = 0, 512, 1024, 2048
O_GQ, O_GK, O_GV, O_GZ = 3072, 3584, 4096, 5120
O_GA, O_GB, O_MA, O_MB = 6144, 6148, 6152, 7176


def _cst_layout():
    names = [("identF", 128), ("ones", 128), ("nones", 128), ("Uc", 128), ("nUc", 128), ("negm", 128),
             ("negmT", 128), ("nbd", 128), ("lom", 128), ("DTr", 512), ("kdec", 4), ("epsr", 4),
             ]
    off = {}
    o = 0
    for n, w in names:
        off[n] = (o, w)
        o += w
    return off, o


CST_OFF, CST_N = _cst_layout()


def make_consts():
    c = np.zeros((128, CST_N), np.float32)

    def put(name, arr):
        o, w = CST_OFF[name]
        c[:arr.shape[0], o:o + w] = arr

    i = np.arange(128)
    put("identF", np.eye(128, dtype=np.float32))
    put("ones", np.ones((128, 128), np.float32))
    put("nones", -np.ones((128, 128), np.float32))
    Uc = (i[:, None] <= i[None, :]).astype(np.float32)
    put("Uc", Uc)
    put("nUc", -Uc)
    negm = np.where(i[None, :] <= i[:, None], 0.0, -30000.0).astype(np.float32)
    put("negm", negm)
    put("negmT", negm.T.copy())
    nbd = np.where((i[:, None] > i[None, :]) & ((i[:, None] // 64) == (i[None, :] // 64)), -1.0, 0.0)
    put("nbd", nbd.astype(np.float32))
    lom = np.where((i[:, None] >= 64) & (i[None, :] < 64), 1.0, 0.0)
    put("lom", lom.astype(np.float32))
    dtr = np.zeros((128, 512), np.float64)
    kdec = np.zeros((128, 4), np.float64)
    epsr = np.zeros((128, 4), np.float64)
    for h in range(4):
        gam = 1.0 - 2.0 ** (-5.0 - h)
        dtr[:, h * 128:(h + 1) * 128] = (gam ** (-(i[:, None] + 1.0))) * DK_SCALE * (i[None, :] >= i[:, None])
        kdec[:, h] = gam ** (127.0 - i) * DK_SCALE
        epsr[:, h] = 1e-6 * gam ** (-2.0 * (i + 1.0))
    put("DTr", dtr.astype(np.float32))
    put("kdec", kdec.astype(np.float32))
    put("epsr", epsr.astype(np.float32))
    half = 64
    inv_freq = (1.0 / (np.float32(10000.0) ** (np.arange(half, dtype=np.float32) / np.float32(half)))).astype(np.float32)
    ang = (np.arange(SEQ, dtype=np.float32)[:, None] * inv_freq[None, :]).astype(np.float32)
    cos = np.cos(ang).astype(np.float32).T
    sin = np.sin(ang).astype(np.float32).T
    rope = np.stack([np.concatenate([cos, cos], 0), np.concatenate([-sin, sin], 0)], 1)
    return c, np.ascontiguousarray(rope.astype(np.float32))


def _prm_layout():
    names = [("nmw", 8), ("nfw", 8), ("now", 8), ("modb", 48), ("convw", 64), ("gnw", 256), ("alog", 4),
             ("dtb", 4), ("rbias", 20), ("Wrt", 160), ("cT", 16)]
    off = {}
    o = 0
    for n, w in names:
        off[n] = (o, w)
        o += w
    return off, o


PRM_OFF, PRM_N = _prm_layout()


class Reg:
    __slots__ = ("w", "r", "excl")

    def __init__(self, excl=False):
        self.w = None
        self.r = {}
        self.excl = excl


class Q:
    def __init__(self, name, eng, sem, is_pe=False):
        self.name, self.eng, self.sem, self.n, self.seen, self.is_pe = name, eng, sem, 0, {}, is_pe


class DSem:
    def __init__(self, sem):
        self.sem, self.n = sem, 0


def emit(q, fn, reads=(), writes=(), dsem=None):
    need = {}
    ex = [r for r in reads if r.excl and r not in writes]
    if ex:
        writes = list(writes) + ex

    def add(tok):
        sem, val, src = tok
        if src is q and q.is_pe:
            return
        k = id(sem)
        if k not in need or need[k][1] < val:
            need[k] = (sem, val)

    for r in reads:
        if r.w is not None:
            add(r.w)
    for w in writes:
        if w.w is not None:
            add(w.w)
        for t in w.r.values():
            add(t)
    for k, (sem, val) in need.items():
        if q.seen.get(k, 0) >= val:
            continue
        q.eng.wait_ge(sem, val)
        q.seen[k] = val
    ins = fn()
    if dsem is None:
        q.n += 1
        ins.then_inc(q.sem, 1)
        tok = (q.sem, q.n, q)
    else:
        dsem.n += 16
        ins.then_inc(dsem.sem, 16)
        tok = (dsem.sem, dsem.n, None)
    for r in reads:
        k = id(tok[0])
        r.r[k] = tok
    for w in writes:
        w.w = tok
        w.r = {}
    return tok


class Coop:
    def __init__(self):
        self.local = threading.local()

    def yield_(self):
        t = getattr(self.local, "task", None)
        if t is None:
            return
        t.main.release()
        t.go.acquire()

    def run(self, fns, depth):
        if depth <= 1:
            for f in fns:
                f()
            return

        class T:
            pass

        pending = list(fns)
        active = []

        def start(fn):
            t = T()
            t.go = threading.Semaphore(0)
            t.main = threading.Semaphore(0)
            t.done = False
            t.exc = None

            def body():
                self.local.task = t
                t.go.acquire()
                try:
                    fn()
                except BaseException as ex:
                    t.exc = ex
                t.done = True
                t.main.release()

            t.th = threading.Thread(target=body, daemon=True)
            t.th.start()
            return t

        while pending or active:
            while len(active) < depth and pending:
                active.append(start(pending.pop(0)))
            for t in list(active):
                t.go.release()
                t.main.acquire()
                if t.exc is not None:
                    raise t.exc
                if t.done:
                    active.remove(t)


class Tile:
    def __init__(self, nc, name, shape, dtype, psum=False, at=None):
        if at is not None:
            self.t = nc.alloc_sbuf_tensor_at("t_" + name, shape, dtype, offset=at)
        else:
            self.t = (nc.alloc_psum_tensor if psum else nc.alloc_sbuf_tensor)("t_" + name, shape, dtype)
        self.name = name
        self.psum = psum
        self.regs = {}

    def r(self, key=None):
        if key not in self.regs:
            self.regs[key] = Reg(excl=self.psum)
        return self.regs[key]

    def __getitem__(self, idx):
        return self.t[idx]


class Arena:
    def __init__(self, off, size):
        self.off, self.size, self.cur = off, size, 0

    def take(self, nbytes):
        nbytes = (nbytes + 31) // 32 * 32
        assert self.cur + nbytes <= self.size, ("arena overflow", self.cur, nbytes, self.size)
        o = self.off + self.cur
        self.cur += nbytes
        return o


class Pool_:
    def __init__(self, nc, name, shape, dtype, n, arena=None, n_arena=0, esize=4):
        self.tiles = [Tile(nc, f"{name}{i}", shape, dtype) for i in range(n)]
        per = esize
        for s_ in shape[1:]:
            per *= s_
        for i in range(n_arena):
            self.tiles.append(Tile(nc, f"{name}x{i}", shape, dtype, at=arena.take(per)))
        self.i = 0

    def next(self):
        t = self.tiles[self.i % len(self.tiles)]
        self.i += 1
        return t


PS_GRAN = [512] * 8
GDN_DEPTH = 4
BANKSETS = {1: [(4, 5, 6, 7)] * 4, 2: [(4, 5, 6, 7), (0, 1, 2, 3)] * 2,
            4: [(2 * t, 2 * t + 1, 2 * t, 2 * t + 1) for t in range(4)]}


def build_program(stages=("mix_ret", "mix_gdn", "moe"), n_pass=2 * SPC):
    nc = bass.Bass("TRN2", target_bir_lowering=False)
    NTOK = SPC * SEQ
    x_d = nc.dram_tensor("x", [NTOK, DM], F32, kind="ExternalInput").ap()
    cst_d = nc.dram_tensor("cst", [128, CST_N], F32, kind="ExternalInput").ap()
    rope_d = nc.dram_tensor("rope", [128, 2, SEQ], F32, kind="ExternalInput").ap()
    prm_d = nc.dram_tensor("prm", [128, PRM_N], F32, kind="ExternalInput").ap()
    modw_d = nc.dram_tensor("modw", [DM, 6 * DM], F32, kind="ExternalInput").ap()
    wj_d = nc.dram_tensor("wj", [8, DM, WCOLS], F32, kind="ExternalInput").ap()
    wo_d = nc.dram_tensor("wo", [8, 256, DM], F32, kind="ExternalInput").ap()
    wg_d = nc.dram_tensor("wg", [16, DM, 512], F32, kind="ExternalInput").ap()
    wu_d = nc.dram_tensor("wu", [16, DM, 512], F32, kind="ExternalInput").ap()
    wd_d = nc.dram_tensor("wd", [16, 512, DM], F32, kind="ExternalInput").ap()
    y_d = nc.dram_tensor("y", [NTOK, DM], F32, kind="ExternalOutput").ap()

    PE = Q("pe", nc.tensor, nc.alloc_semaphore("s_pe"), is_pe=True)
    ACT = Q("act", nc.scalar, nc.alloc_semaphore("s_act"))
    DVE = Q("dve", nc.vector, nc.alloc_semaphore("s_dve"))
    POOL = Q("pool", nc.gpsimd, nc.alloc_semaphore("s_pool"))
    SP = Q("sp", nc.sync, nc.alloc_semaphore("s_sp"))
    dsem_cache = {}

    def dsem(name):
        if name not in dsem_cache:
            dsem_cache[name] = DSem(nc.alloc_semaphore("d_" + name))
        return dsem_cache[name]

    def flat(lst):
        out = []
        for a in lst:
            if isinstance(a, (list, tuple)):
                out.extend(flat(a))
            else:
                out.append(a)
        return out

    coop = Coop()

    def E(q, fn, reads, writes, dsem_=None):
        tok = emit(q, fn, flat(reads), flat(writes), dsem=dsem_)
        coop.yield_()
        return tok

    def dma(q, out, in_, reads, writes, sem):
        return E(q, lambda: q.eng.dma_start(out=out, in_=in_), reads, writes, dsem(sem))

    def mm(out, lhsT, rhs, start, stop, reads, writes):
        return E(PE, lambda: nc.tensor.matmul(out, lhsT, rhs, start=start, stop=stop), reads, writes)

    def tr(out, in_, ident, reads, writes):
        return E(PE, lambda: nc.tensor.transpose(out, in_, ident), reads, writes)

    def act(out, in_, func, reads, writes, bias=None, scale=None, accum=None):
        kw = {}
        if bias is not None:
            kw["bias"] = bias
        if scale is not None:
            kw["scale"] = scale
        if accum is not None:
            kw["accum_out"] = accum
        return E(ACT, lambda: nc.scalar.activation(out=out, in_=in_, func=func, **kw), reads, writes)

    def tt(q, out, in0, in1, op, reads, writes):
        return E(q, lambda: q.eng.tensor_tensor(out=out, in0=in0, in1=in1, op=op), reads, writes)

    def ts(q, out, in0, s1, s2, op0, op1, reads, writes):
        if s2 is None:
            return E(q, lambda: q.eng.tensor_scalar(out=out, in0=in0, scalar1=s1, scalar2=None, op0=op0), reads, writes)
        return E(q, lambda: q.eng.tensor_scalar(out=out, in0=in0, scalar1=s1, scalar2=s2, op0=op0, op1=op1), reads, writes)

    def stt(out, in0, scalar, in1, op0, op1, reads, writes, accum=None):
        if accum is not None:
            return E(DVE, lambda: nc.vector.scalar_tensor_tensor(out=out, in0=in0, scalar=scalar, in1=in1, op0=op0, op1=op1, accum_out=accum), reads, writes)
        return E(DVE, lambda: nc.vector.scalar_tensor_tensor(out=out, in0=in0, scalar=scalar, in1=in1, op0=op0, op1=op1), reads, writes)

    def cp(q, out, in_, reads, writes):
        if q is ACT:
            return E(q, lambda: nc.scalar.activation(out=out, in_=in_, func=AF.Copy), reads, writes)
        return E(q, lambda: q.eng.tensor_copy(out=out, in_=in_), reads, writes)

    def memset(q, ap, val, writes):
        return E(q, lambda: q.eng.memset(ap, val), [], writes)

    def pw(out, in0, in1, reads, writes):
        return E(POOL, lambda: nc.gpsimd.tensor_tensor(out=out, in0=in0, in1=in1, op=ALU.pow), reads, writes)

    cst = Tile(nc, "cst", [128, CST_N], F32)
    prm = Tile(nc, "prm", [128, PRM_N], F32)
    cr, pr_ = cst.r(), prm.r()

    def C(name, lo=0, hi=None, rows=128):
        o, w = CST_OFF[name]
        hi = w if hi is None else hi
        return cst[0:rows, o + lo:o + hi]

    def Pm(name, lo=0, hi=None):
        o, w = PRM_OFF[name]
        hi = w if hi is None else hi
        return prm[:, o + lo:o + hi]

    identB = Tile(nc, "identB", [128, 128], BF16)
    nhalf = Tile(nc, "nhalf", [128, 8], F32)
    xT = Tile(nc, "xT", [128, 8, TOK], F32)
    xmT = Tile(nc, "xmT", [128, 8, TOK], BF16)
    WA = [Tile(nc, f"WA{i}", [128, 6144], F32) for i in range(2)]
    combT = Tile(nc, "combT", [16, TOK], F32)
    misc = Tile(nc, "misc", [128, 256], F32)
    SfA = Tile(nc, "SfA", [128, 8, 256], F32)
    HAL = Tile(nc, "HAL", [128, 16, 4], F32)
    Sb = Tile(nc, "Sb", [128, 256], BF16)
    PSt = [Tile(nc, f"ps{i}", [128, 512], F32, psum=True) for i in range(8)]

    def PR(bank, c0=0, c1=512):
        g = PS_GRAN[bank]
        return [PSt[bank].r(k) for k in range(c0 // g, (c1 - 1) // g + 1)]

    def PSf(bank, c0=0, c1=512, rows=128):
        return PSt[bank][0:rows, c0:c1]

    def psb(bank, c0, c1):
        return PSt[bank].t[:].bitcast(BF16)[:, 2 * c0:2 * c1]

    def WAf(slot):
        return WA[slot].t[:].rearrange("p (k n) -> p k n", k=8)

    def WAb(slot):
        return WA[slot].t[:].bitcast(BF16)

    ARENA_BYTES = 22528
    _base = (nc.sbuf_base + 31) // 32 * 32
    _slab = nc.alloc_sbuf_tensor("t_arena", [128, ARENA_BYTES // 4], F32)
    arA = Arena(_base, ARENA_BYTES)
    arB = Arena(_base, ARENA_BYTES)
    XIN = Pool_(nc, "xin", [128, 1024], F32, 0, arena=arA, n_arena=2)
    ROPE = Pool_(nc, "rope", [128, 2, 512], F32, 1)
    F512 = Pool_(nc, "f512", [128, 512], F32, 4)
    OB = Pool_(nc, "ob", [128, 512], F32, 0, arena=arA, n_arena=2)
    RS = Pool_(nc, "rs", [128, 512], F32, 2)
    CBS = Pool_(nc, "cbs", [128, 512], F32, 0, arena=arA, n_arena=1)
    DECT = Pool_(nc, "dect", [128, 128], F32, 3, arena=arB, n_arena=2)
    LO = Pool_(nc, "lo", [128, 128], F32, 3, arena=arB, n_arena=2)
    B512 = Pool_(nc, "b512", [128, 512], BF16, 6)
    F256 = Pool_(nc, "f256", [128, 256], F32, 8, arena=arB, n_arena=6)
    B256 = Pool_(nc, "b256", [128, 256], BF16, 8, arena=arB, n_arena=4, esize=2)
    F128 = Pool_(nc, "f128", [128, 128], F32, 16, arena=arB, n_arena=12)
    B128 = Pool_(nc, "b128", [128, 128], BF16, 20, arena=arB, n_arena=16, esize=2)
    GATE = Pool_(nc, "gate", [128, 256], F32, 3, arena=arB, n_arena=2)
    SM = Pool_(nc, "sm", [128, 32], F32, 8)
    YT = Pool_(nc, "yT", [128, 2, 512], BF16, 2)
    ACTT = Pool_(nc, "actT", [128, 4, 512], BF16, 0, arena=arA, n_arena=2, esize=2)
    CB = [Tile(nc, f"cb{c}", [128, 516], F32) for c in range(4)]

    def full_barrier():
        qs = (PE, ACT, DVE, POOL, SP)
        targets = [(p.sem, p.n, p) for p in qs] + [(d_.sem, d_.n, None) for d_ in dsem_cache.values()]
        for q in qs:
            for (sem, val, p) in targets:
                if p is q or val == 0:
                    continue
                if val > q.seen.get(id(sem), 0):
                    q.eng.wait_ge(sem, val)
                    q.seen[id(sem)] = val

    def pe_barrier():
        for q in (ACT, DVE, POOL):
            if q.n > PE.seen.get(id(q.sem), 0):
                nc.tensor.wait_ge(q.sem, q.n)
                PE.seen[id(q.sem)] = q.n

    dma(SP, cst[:, :], cst_d[:, :], [], [cr], "cst")
    dma(SP, prm[:, :], prm_d[:, :], [], [pr_], "prm")
    cp(DVE, identB[:, :], C("identF"), [cr], [identB.r()])
    memset(DVE, nhalf[:, :], -0.5, [nhalf.r()])
    mr = misc.r()
    scT = misc[:, 0:16]
    act(scT, Pm("cT"), AF.Silu, [pr_], [mr])
    act(misc[:, 16:20], Pm("alog"), AF.Exp, [pr_], [mr])
    ts(DVE, misc[:, 20:24], misc[:, 16:20], -1.0, None, ALU.mult, None, [mr], [mr])
    NEA = misc[:, 20:24]
    memset(DVE, misc[:, 24:25], 1e-6, [mr])
    memset(DVE, misc[:, 25:26], 128.0e-6, [mr])
    EPSB = misc[:, 24:26]

    modw_v = modw_d.rearrange("(kc p) n -> p kc n", p=128)
    wslot = [0]
    for cbk in range(8):
        slot = wslot[0] % 2
        wslot[0] += 1
        dma(SP, WAf(slot), modw_v[:, :, cbk * 768:(cbk + 1) * 768], [], [WA[slot].r()], f"mw{slot}")
        for j in range(6):
            jb = cbk * 6 + j
            for kc in range(8):
                mm(PSf(0, jb * 2, jb * 2 + 2), WAf(slot)[:, kc, j * 128:(j + 1) * 128], scT[:, kc * 2:kc * 2 + 2],
                   kc == 0, kc == 7, [WA[slot].r(), mr], [PR(0)])
    ps3 = PSf(0, 0, 96).rearrange("p (j b) -> p j b", b=2)
    for b in range(2):
        tt(DVE, misc[:, 32 + 48 * b:80 + 48 * b], ps3[:, :, b], Pm("modb"), ALU.add, [PR(0), pr_], [mr])

    def modv(b, k):
        return misc[:, 32 + 48 * b + 8 * k:32 + 48 * b + 8 * k + 8]

    for b in range(2):
        for (dst, k, nw) in ((128 + 8 * b, 1, "nmw"), (144 + 8 * b, 4, "nfw")):
            ts(DVE, misc[:, dst:dst + 8], modv(b, k), 1.0, None, ALU.add, None, [mr], [mr])
            tt(DVE, misc[:, dst:dst + 8], misc[:, dst:dst + 8], Pm(nw), ALU.mult, [mr, pr_], [mr])

    def A1(b):
        return misc[:, 128 + 8 * b:136 + 8 * b]

    def A2(b):
        return misc[:, 144 + 8 * b:152 + 8 * b]

    def xr(dc, blk):
        return xT.r((dc, blk))

    def xmr(dc, blk):
        return xmT.r((dc, blk))

    def BS(blk):
        return slice(blk * 512, (blk + 1) * 512)

    def phase_load(row0):
        for t in range(TOK // 128):
            xin = XIN.next()
            dma(SP, xin[:, :], x_d[row0 + t * 128:row0 + (t + 1) * 128, :], [], [xin.r()], xin.name)
            for half in range(2):
                bank = (2 * t + half) % 4
                for j in range(4):
                    dc = half * 4 + j
                    tr(PSf(bank, j * 128, (j + 1) * 128), xin[:, dc * 128:(dc + 1) * 128], C("identF"),
                       [xin.r(), cr], [PR(bank)])
                q = ACT if half == 0 else DVE
                cp(q, xT[:, half * 4:half * 4 + 4, t * 128:(t + 1) * 128],
                   PSf(bank).rearrange("p (j c) -> p j c", j=4), [PR(bank)],
                   [xr(half * 4 + j, t // 4) for j in range(4)])

    def rstd_block(blk):
        for dc in range(8):
            sq = F512.next()
            act(sq[:, :], xT[:, dc, BS(blk)], AF.Square, [xr(dc, blk)], [sq.r()])
            mm(PSf(4), C("ones"), sq[:, :], dc == 0, dc == 7, [cr, sq.r()], [PR(4)])
        t1 = F512.next()
        act(t1[:, :], PSf(4), AF.Ln, [PR(4), mr], [t1.r()], bias=EPSB[:, 0:1], scale=1.0 / DM)
        rs = RS.next()
        act(rs[:, :], t1[:, :], AF.Exp, [t1.r()], [rs.r()], scale=-0.5)
        return rs

    def phase_norm_mix(b):
        for blk in range(NBLK):
            rs = rstd_block(blk)
            for dc in range(8):
                tmp = F512.next()
                stt(tmp[:, :], xT[:, dc, BS(blk)], A1(b)[:, dc:dc + 1], rs[:, :], ALU.mult, ALU.mult,
                    [xr(dc, blk), mr, rs.r()], [tmp.r()])
                act(xmT[:, dc, BS(blk)], tmp[:, :], AF.Identity, [tmp.r(), mr], [xmr(dc, blk)],
                    bias=modv(b, 0)[:, dc:dc + 1])

    def routing(tlg, bank):
        l = SM.next()
        lr = l.r()
        tt(DVE, l[:, 0:20], PSf(bank, 0, 20), Pm("rbias"), ALU.add, [PR(bank, 0, 20), pr_], [lr])
        w = SM.next()
        wr = w.r()
        E(DVE, lambda: nc.vector.tensor_reduce(out=w[:, 0:1], in_=l[:, 0:4], axis=AX.X, op=ALU.max), [lr], [wr])
        ts(DVE, w[:, 1:2], w[:, 0:1], -1.0, None, ALU.mult, None, [wr], [wr])
        act(w[:, 22:26], l[:, 0:4], AF.Exp, [lr, wr], [wr], bias=w[:, 1:2], accum=w[:, 2:3])
        E(DVE, lambda: nc.vector.reciprocal(out=w[:, 3:4], in_=w[:, 2:3]), [wr], [wr])
        ts(DVE, w[:, 4:8], l[:, 0:4], w[:, 0:1], None, ALU.is_equal, None, [lr, wr], [wr])
        ts(DVE, w[:, 8:12], l[:, 4:8], w[:, 4:5], None, ALU.mult, None, [lr, wr], [wr])
        for g in range(1, 4):
            stt(w[:, 8:12], l[:, 4 + 4 * g:8 + 4 * g], w[:, 4 + g:5 + g], w[:, 8:12], ALU.mult, ALU.add, [lr, wr], [wr])
        E(DVE, lambda: nc.vector.tensor_reduce(out=w[:, 12:13], in_=w[:, 8:12], axis=AX.X, op=ALU.max), [wr], [wr])
        ts(DVE, w[:, 13:14], w[:, 12:13], -1.0, None, ALU.mult, None, [wr], [wr])
        ts(DVE, w[:, 14:18], w[:, 8:12], w[:, 12:13], None, ALU.is_equal, None, [wr], [wr])
        stt(w[:, 14:18], w[:, 14:18], -1e30, w[:, 8:12], ALU.mult, ALU.add, [wr], [wr])
        E(DVE, lambda: nc.vector.tensor_reduce(out=w[:, 18:19], in_=w[:, 14:18], axis=AX.X, op=ALU.max), [wr], [wr])
        ts(DVE, w[:, 14:18], w[:, 8:12], w[:, 18:19], None, ALU.is_ge, None, [wr], [wr])
        act(w[:, 22:26], w[:, 8:12], AF.Exp, [wr], [wr], bias=w[:, 13:14])
        stt(w[:, 26:30], w[:, 22:26], 1.0, w[:, 14:18], ALU.mult, ALU.mult, [wr], [wr], accum=w[:, 19:20])
        E(DVE, lambda: nc.vector.reciprocal(out=w[:, 20:21], in_=w[:, 19:20]), [wr], [wr])
        tt(DVE, w[:, 21:22], w[:, 3:4], w[:, 20:21], ALU.mult, [wr], [wr])
        cm = SM.next()
        for g in range(4):
            ts(DVE, cm[:, 4 * g:4 * g + 4], w[:, 26:30], w[:, 4 + g:5 + g], w[:, 21:22], ALU.mult, ALU.mult, [wr], [cm.r()])
        tr(PSf(5, 0, 128, rows=16), cm[:, 0:16], C("identF"), [cm.r(), cr], [PR(5, 0, 128)])
        cp(ACT, combT[0:16, tlg * 128:(tlg + 1) * 128], PSf(5, 0, 128, rows=16), [PR(5, 0, 128)], [combT.r(tlg // 4)])

    def phase_norm_ffn(b):
        WrtF = Pm("Wrt").rearrange("p (k n) -> p k n", k=8)
        for blk in range(NBLK):
            rs = rstd_block(blk)
            for dc in range(8):
                tmp = F512.next()
                stt(tmp[:, :], xT[:, dc, BS(blk)], A2(b)[:, dc:dc + 1], rs[:, :], ALU.mult, ALU.mult,
                    [xr(dc, blk), mr, rs.r()], [tmp.r()])
                t2 = F512.next()
                act(t2[:, :], tmp[:, :], AF.Identity, [tmp.r(), mr], [t2.r()], bias=modv(b, 3)[:, dc:dc + 1])
                cp(DVE, xmT[:, dc, BS(blk)], t2[:, :], [t2.r()], [xmr(dc, blk)])
                for tl in range(4):
                    mm(PSf(tl, 0, 20), t2[:, tl * 128:(tl + 1) * 128], WrtF[:, dc, :], dc == 0, dc == 7,
                       [t2.r(), pr_], [PR(tl, 0, 20)])
            for tl in range(4):
                routing(blk * 4 + tl, tl)

    def load_expert(e):
        slot = wslot[0] % 2
        wslot[0] += 1
        wb = WAb(slot)
        rg = WA[slot].r()
        dma(POOL, wb[:, 0:4096].rearrange("p (k n) -> p k n", k=8), wg_d[e].rearrange("(kc p) f -> p kc f", p=128), [], [rg], f"wa{slot}")
        dma(POOL, wb[:, 4096:8192].rearrange("p (k n) -> p k n", k=8), wu_d[e].rearrange("(kc p) f -> p kc f", p=128), [], [rg], f"wa{slot}")
        dma(POOL, wb[:, 8192:12288].rearrange("p (k n) -> p k n", k=4), wd_d[e].rearrange("(fc p) d -> p fc d", p=128), [], [rg], f"wa{slot}")
        return slot

    def phase_moe(b, slots):
        gf = modv(b, 5)

        def views(e):
            wb = WAb(slots[e])
            return (WA[slots[e]].r(), wb[:, 0:4096].rearrange("p (k n) -> p k n", k=8),
                    wb[:, 4096:8192].rearrange("p (k n) -> p k n", k=8),
                    wb[:, 8192:12288].rearrange("p (k n) -> p k n", k=4))

        def gate_up(e, blk):
            rg, Wg, Wu, Wd = views(e)
            bs = BS(blk)
            cme = F512.next()
            ts(DVE, cme[0:16, :], combT[0:16, bs], C("identF", e, e + 1, rows=16), None, ALU.mult, None,
               [combT.r(blk), cr], [cme.r()])
            mm(PSf(6), C("ones", rows=16), cme[0:16, :], True, True, [cr, cme.r()], [PR(6)])
            cbs = CBS.next()
            cp(ACT, cbs[:, :], PSf(6), [PR(6)], [cbs.r()])
            aT = ACTT.next()
            for fc in range(4):
                pg, pu = 2 * (fc % 2), 2 * (fc % 2) + 1
                for dc in range(8):
                    mm(PSf(pg), Wg[:, dc, fc * 128:(fc + 1) * 128], xmT[:, dc, bs], dc == 0, dc == 7, [rg, xmr(dc, blk)], [PR(pg)])
                for dc in range(8):
                    mm(PSf(pu), Wu[:, dc, fc * 128:(fc + 1) * 128], xmT[:, dc, bs], dc == 0, dc == 7, [rg, xmr(dc, blk)], [PR(pu)])
                sg = F512.next()
                act(sg[:, :], PSf(pg), AF.Silu, [PR(pg)], [sg.r()])
                t = F512.next()
                tt(DVE, t[:, :], PSf(pu), sg[:, :], ALU.mult, [PR(pu), sg.r()], [t.r()])
                tt(POOL, aT[:, fc, :], t[:, :], cbs[:, :], ALU.mult, [t.r(), cbs.r()], [aT.r(fc)])
            return aT

        def down(e, blk, aT):
            rg, Wg, Wu, Wd = views(e)
            bs = BS(blk)
            for dc in range(8):
                po = (4, 5, 7)[dc % 3]
                for fc in range(4):
                    mm(PSf(po), Wd[:, fc, dc * 128:(dc + 1) * 128], aT[:, fc, :], fc == 0, fc == 3, [rg, aT.r(fc)], [PR(po)])
                stt(xT[:, dc, bs], PSf(po), gf[:, dc:dc + 1], xT[:, dc, bs], ALU.mult, ALU.add, [PR(po), mr, xr(dc, blk)], [xr(dc, blk)])

        units = [(e, blk) for e in range(16) for blk in range(NBLK)]
        prev = None
        for (e, blk) in units:
            aT = gate_up(e, blk)
            if prev is not None:
                pe_, pblk, paT = prev
                down(pe_, pblk, paT)
                if pblk == NBLK - 1 and pe_ + 2 < 16:
                    slots[pe_ + 2] = load_expert(pe_ + 2)
            prev = (e, blk, aT)
        down(*prev)

    def load_job(job):
        ncols = WCOLS if job < 4 else 1028
        slot = wslot[0] % 2
        wslot[0] += 1
        wb = WAb(slot)
        rg = WA[slot].r()
        dma(POOL, wb[:, 0:10240].rearrange("p (k n) -> p k n", k=8)[:, :, 0:ncols],
            wj_d[job].rearrange("(kc p) n -> p kc n", p=128)[:, :, 0:ncols], [], [rg], f"wa{slot}")
        dma(POOL, wb[:, 10240:12288].rearrange("p (k n) -> p k n", k=2), wo_d[job].rearrange("(cc p) d -> p cc d", p=128), [], [rg], f"wa{slot}")
        return slot

    def out_proj(slot, yT, blk, gm):
        rg = WA[slot].r()
        Wo = WAb(slot)[:, 10240:12288].rearrange("p (k n) -> p k n", k=2)
        for dc in range(8):
            po = dc % 4
            for cc in range(2):
                mm(PSf(po), Wo[:, cc, dc * 128:(dc + 1) * 128], yT[:, cc, :], cc == 0, cc == 1, [rg, yT.r()], [PR(po)])
            stt(xT[:, dc, BS(blk)], PSf(po), gm[:, dc:dc + 1], xT[:, dc, BS(blk)], ALU.mult, ALU.add, [PR(po), mr, xr(dc, blk)], [xr(dc, blk)])

    def y_transposes(y, yT, tsl, bk=7):
        for cc in range(2):
            tr(psb(bk, 64 * cc, 64 * cc + 64), y[:, cc * 128:(cc + 1) * 128], identB[:, :], [y.r(), identB.r()], [PR(bk, 0, 128)])
        cp(ACT, yT[:, 0:2, tsl], psb(bk, 0, 128).rearrange("p (c t) -> p c t", c=2), [PR(bk, 0, 128)], [yT.r()])

    def state_init(j, hf):
        sr_ = SfA.r(j)
        if hf == 0:
            memset(DVE, SfA[:, j, :], 0.0, [sr_])
        cp(ACT, Sb[:, :], SfA[:, j, :], [sr_], [Sb.r()])

    def ret_job(b, hf, h, slot):
        gm = modv(b, 2)
        rg = WA[slot].r()
        Wr = WAb(slot)[:, 0:10240].rearrange("p (k n) -> p k n", k=8)
        state_init(h, hf)
        Sf = SfA[:, h, :]
        sfr = SfA.r(h)
        gam128 = float((1.0 - 2.0 ** (-5.0 - h)) ** 128)
        for blk in range(NBLK):
            bs = BS(blk)
            pos0 = hf * TOK + blk * 512
            rp = ROPE.next()
            dma(SP, rp[:, :, :], rope_d[:, :, pos0:pos0 + 512], [], [rp.r()], "rope")
            for i in range(4):
                for dc in range(8):
                    mm(PSf(i), Wr[:, dc, i * 128:(i + 1) * 128], xmT[:, dc, bs], dc == 0, dc == 7, [rg, xmr(dc, blk)], [PR(i)])
            rot = []
            for (pa, pb_) in ((0, 1), (2, 3)):
                t1 = F512.next()
                tt(DVE, t1[:, :], PSf(pa), rp[:, 0, :], ALU.mult, [PR(pa), rp.r()], [t1.r()])
                t2 = F512.next()
                tt(DVE, t2[:, :], PSf(pb_), rp[:, 1, :], ALU.mult, [PR(pb_), rp.r()], [t2.r()])
                o = B512.next()
                tt(POOL, o[:, :], t1[:, :], t2[:, :], ALU.add, [t1.r(), t2.r()], [o.r()])
                rot.append(o)
            qrT, krT = rot
            yT = YT.next()
            chain_done = [0]

            def tile_body(tl):
                bP, bQ, bR, bS = BANKSETS[GDN_DEPTH][tl]
                tsl = slice(tl * 128, (tl + 1) * 128)
                gs = slice(blk * 512 + tl * 128, blk * 512 + (tl + 1) * 128)
                for dc in range(8):
                    mm(PSf(bP), xmT[:, dc, gs], Wr[:, dc, 512:1024], dc == 0, dc == 7, [rg, xmr(dc, blk)], [PR(bP)])
                for dc in range(8):
                    mm(PSf(bQ, 0, 256), xmT[:, dc, gs], Wr[:, dc, 1024:1280], dc == 0, dc == 7, [rg, xmr(dc, blk)], [PR(bQ, 0, 256)])
                vb = B256.next()
                cp(ACT, vb[:, :], PSf(bP, 0, 256), [PR(bP)], [vb.r()])
                sg = F256.next()
                act(sg[:, :], PSf(bP, 256, 512), AF.Silu, [PR(bP)], [sg.r()])
                sa = F256.next()
                act(sa[:, :], PSf(bQ, 0, 256), AF.Sigmoid, [PR(bQ, 0, 256)], [sa.r()])
                gate = GATE.next()
                tt(POOL, gate[:, :], sg[:, :], sa[:, :], ALU.mult, [sg.r(), sa.r()], [gate.r()])
                tr(psb(bQ, 256, 320), krT[:, tsl], identB[:, :], [krT.r(), identB.r()], [PR(bQ, 256, 320)])
                kin = B128.next()
                ts(DVE, kin[:, :], psb(bQ, 256, 320), C("kdec", h, h + 1), None, ALU.mult, None, [PR(bQ, 256, 320), cr], [kin.r()])
                mm(PSf(bQ, 384, 512), krT[:, tsl], qrT[:, tsl], True, True, [krT.r(), qrT.r()], [PR(bQ, 384, 512)])
                sT = B128.next()
                tt(DVE, sT[:, :], PSf(bQ, 384, 512), C("DTr", h * 128, (h + 1) * 128), ALU.mult, [PR(bQ, 384, 512), cr], [sT.r()])
                mm(PSf(bR, 0, 256), sT[:, :], vb[:, :], True, False, [sT.r(), vb.r()], [PR(bR, 0, 256)])
                while chain_done[0] < tl:
                    coop.yield_()
                mm(PSf(bR, 0, 256), qrT[:, tsl], Sb[:, :], False, True, [qrT.r(), Sb.r()], [PR(bR, 0, 256)])
                sm = SM.next()
                junk = F256.next()
                act(junk[:, :], PSf(bR, 0, 256), AF.Square, [PR(bR, 0, 256)], [junk.r(), sm.r()], accum=sm[:, 0:1])
                ts(DVE, sm[:, 1:2], sm[:, 0:1], 1.0 / 256.0, C("epsr", h, h + 1), ALU.mult, ALU.add, [sm.r(), cr], [sm.r()])
                pw(sm[:, 2:3], sm[:, 1:2], nhalf[:, 0:1], [sm.r(), nhalf.r()], [sm.r()])
                y = B256.next()
                stt(y[:, :], PSf(bR, 0, 256), sm[:, 2:3], gate[:, :], ALU.mult, ALU.mult, [PR(bR, 0, 256), sm.r(), gate.r()], [y.r()])
                y_transposes(y, yT, tsl, bS)
                mm(PSf(bR, 256, 512), kin[:, :], vb[:, :], True, True, [kin.r(), vb.r()], [PR(bR, 256, 512)])
                stt(Sf, Sf, gam128, PSf(bR, 256, 512), ALU.mult, ALU.add, [sfr, PR(bR, 256, 512)], [sfr])
                cp(ACT, Sb[:, :], Sf, [sfr], [Sb.r()])
                chain_done[0] = tl + 1

            coop.run([(lambda tl=tl: tile_body(tl)) for tl in range(4)], depth=GDN_DEPTH)
            out_proj(slot, yT, blk, gm)

    def gdn_job(b, hf, h, slot):
        gm = modv(b, 2)
        rg = WA[slot].r()
        Wg = WAb(slot)[:, 0:10240].rearrange("p (k n) -> p k n", k=8)
        j = 4 + h
        state_init(j, hf)
        Sf = SfA[:, j, :]
        sfr = SfA.r(j)
        hr = HAL.r(h)
        if hf == 0:
            memset(DVE, HAL[:, 4 * h:4 * h + 4, :], 0.0, [hr])
        chunks = [h, 4 + h, 8 + 2 * h, 9 + 2 * h]
        cw = Pm("convw")
        IF = C("identF")
        for blk in range(NBLK):
            bs = BS(blk)
            pT = []
            for cc in range(4):
                for dc in range(8):
                    mm(PSf(cc), Wg[:, dc, cc * 128:(cc + 1) * 128], xmT[:, dc, bs], dc == 0, dc == 7, [rg, xmr(dc, blk)], [PR(cc)])
                cb = CB[cc]
                cbr = cb.r()
                cp(DVE, cb[:, 0:3], HAL[:, 4 * h + cc, 0:3], [hr], [cbr])
                cp(ACT, cb[:, 3:515], PSf(cc), [PR(cc)], [cbr])
                cp(DVE, HAL[:, 4 * h + cc, 0:3], cb[:, 512:515], [cbr], [hr])
                acc = F512.next()
                ch = chunks[cc]
                ts(DVE, acc[:, :], cb[:, 0:512], cw[:, ch * 4:ch * 4 + 1], None, ALU.mult, None, [cbr, pr_], [acc.r()])
                for k in range(1, 4):
                    stt(acc[:, :], cb[:, k:k + 512], cw[:, ch * 4 + k:ch * 4 + k + 1], acc[:, :], ALU.mult, ALU.add, [cbr, pr_, acc.r()], [acc.r()])
                p = B512.next()
                act(p[:, :], acc[:, :], AF.Silu, [acc.r()], [p.r()])
                pT.append(p)
            yT = YT.next()
            chain_done = [0]

            def tile_body(tl):
                bP, bQ, bR, bS = BANKSETS[GDN_DEPTH][tl]
                tsl = slice(tl * 128, (tl + 1) * 128)
                gs = slice(blk * 512 + tl * 128, blk * 512 + (tl + 1) * 128)
                for dc in range(8):
                    mm(PSf(bP), xmT[:, dc, gs], Wg[:, dc, 512:1024], dc == 0, dc == 7, [rg, xmr(dc, blk)], [PR(bP)])
                rab = PR(bQ, 0, 64)
                for dc in range(8):
                    mm(PSf(bQ, 0, 2), xmT[:, dc, gs], Wg[:, dc, 1024:1026], dc == 0, dc == 7, [rg, xmr(dc, blk)], [rab])
                sm = SM.next()
                sr = sm.r()
                act(sm[:, 18:19], PSf(bQ, 1, 2), AF.Exp, [rab], [sr], scale=-1.0)
                ts(DVE, sm[:, 18:19], sm[:, 18:19], 1.0, None, ALU.add, None, [sr], [sr])
                E(DVE, lambda: nc.vector.reciprocal(out=sm[:, 0:1], in_=sm[:, 18:19]), [sr], [sr])
                act(sm[:, 1:2], PSf(bQ, 0, 1), AF.Exp, [rab, pr_], [sr], bias=Pm("dtb", h, h + 1))
                act(sm[:, 2:3], sm[:, 1:2], AF.Ln, [sr], [sr], bias=1.0)
                ts(DVE, sm[:, 3:4], sm[:, 2:3], NEA[:, h:h + 1], None, ALU.mult, None, [sr, mr], [sr])
                ts(DVE, sm[:, 4:5], sm[:, 2:3], NEA[:, h:h + 1], None, ALU.mult, None, [sr, mr], [sr])
                ee = F512.next()
                act(ee[:, :], PSf(bP), AF.Exp, [PR(bP)], [ee.r()], scale=-1.0)
                ts(DVE, ee[:, 256:512], ee[:, 256:512], 1.0, None, ALU.add, None, [ee.r()], [ee.r()])
                stt(ee[:, 0:256], ee[:, 0:256], 1.0, ee[:, 256:512], ALU.add, ALU.mult, [ee.r()], [ee.r()])
                sz = F256.next()
                E(DVE, lambda: nc.vector.reciprocal(out=sz[:, :], in_=ee[:, 0:256]), [ee.r()], [sz.r()])
                g3 = F256.next()
                tt(DVE, g3[:, :], PSf(bP, 0, 256), Pm("gnw"), ALU.mult, [PR(bP), pr_], [g3.r()])
                gate = GATE.next()
                tt(POOL, gate[:, :], g3[:, :], sz[:, :], ALU.mult, [g3.r(), sz.r()], [gate.r()])
                res = []
                for (idx, c_ssq, c_r, mulv, addv) in ((0, 5, 6, 128.0, 128.0e-6), (1, 7, 8, 1.0, 1e-6)):
                    ca = 64 + 64 * idx
                    ra = PR(bQ, ca, ca + 64)
                    pa = psb(bQ, ca, ca + 64)
                    tr(pa, pT[idx][:, tsl], identB[:, :], [pT[idx].r(), identB.r()], [ra])
                    junk = F128.next()
                    act(junk[:, :], pa, AF.Square, [ra], [junk.r(), sr], accum=sm[:, c_ssq:c_ssq + 1])
                    act(sm[:, 17:18], sm[:, c_ssq:c_ssq + 1], AF.Ln, [sr, mr], [sr], bias=EPSB[:, 1 - idx:2 - idx], scale=mulv)
                    act(sm[:, c_r:c_r + 1], sm[:, 17:18], AF.Exp, [sr], [sr], scale=-0.5)
                    n_tok = B128.next()
                    ts(DVE, n_tok[:, :], pa, sm[:, c_r:c_r + 1], None, ALU.mult, None, [ra, sr], [n_tok.r()])
                    cb2 = 192 + 64 * idx
                    rb = PR(bQ, cb2, cb2 + 64)
                    pb2 = psb(bQ, cb2, cb2 + 64)
                    tr(pb2, n_tok[:, :], identB[:, :], [n_tok.r(), identB.r()], [rb])
                    nT = B128.next()
                    cp(ACT, nT[:, :], pb2, [rb], [nT.r()])
                    res.append((n_tok, nT))
                (qn, qnT), (kn, knT) = res
                rv = PR(bQ, 320, 448)
                for c2 in range(2):
                    tr(psb(bQ, 320 + 64 * c2, 384 + 64 * c2), pT[2 + c2][:, tsl], identB[:, :], [pT[2 + c2].r(), identB.r()], [rv])
                vb = B256.next()
                ts(DVE, vb[:, :], psb(bQ, 320, 448), sm[:, 0:1], None, ALU.mult, None, [rv, sr], [vb.r()])
                gbc = F128.next()
                ts(DVE, gbc[:, :], C("ones"), sm[:, 3:4], None, ALU.mult, None, [cr, sr], [gbc.r()])
                gU = F128.next()
                ts(DVE, gU[:, :], C("Uc"), sm[:, 3:4], None, ALU.mult, None, [cr, sr], [gU.r()])
                rD, rDT, rG = PR(bR, 0, 128), PR(bR, 128, 256), PR(bQ, 448, 512)
                mm(PSf(bR, 0, 128), gU[:, :], C("ones"), True, False, [gU.r(), cr], [rD])
                mm(PSf(bR, 0, 128), gbc[:, :], C("nUc"), False, True, [gbc.r(), cr], [rD])
                mm(PSf(bR, 128, 256), gU[:, :], C("nones"), True, False, [gU.r(), cr], [rDT])
                mm(PSf(bR, 128, 256), gbc[:, :], C("Uc"), False, True, [gbc.r(), cr], [rDT])
                mm(PSf(bQ, 448, 450), C("Uc"), sm[:, 3:5], True, True, [cr, sr], [rG])
                mm(PSf(bQ, 452, 454), C("ones"), sm[:, 3:5], True, True, [cr, sr], [rG])
                decs = []
                for (c0, nm, rr) in ((0, "negm", rD), (128, "negmT", rDT)):
                    d0 = F128.next()
                    stt(d0[:, :], PSf(bR, c0, c0 + 128), 0.0, C(nm), ALU.min, ALU.add, [rr, cr], [d0.r()])
                    d1 = F128.next() if c0 == 0 else DECT.next()
                    act(d1[:, :], d0[:, :], AF.Exp, [d0.r()], [d1.r()])
                    decs.append(d1)
                dec, decT = decs
                act(sm[:, 9:10], PSf(bQ, 448, 449), AF.Exp, [rG], [sr])
                cp(DVE, sm[:, 10:11], PSf(bQ, 452, 453), [rG], [sr])
                act(sm[:, 11:12], PSf(bQ, 452, 453), AF.Exp, [rG], [sr])
                act(sm[:, 12:13], PSf(bQ, 448, 449), AF.Exp, [rG, sr], [sr], bias=sm[:, 10:11], scale=-1.0)
                tt(DVE, sm[:, 13:14], sm[:, 0:1], sm[:, 9:10], ALU.mult, [sr], [sr])
                rKK = PR(bS, 0, 128)
                mm(PSf(bS, 0, 128), knT[:, :], knT[:, :], True, True, [knT.r()], [rKK])
                ndbd = F128.next()
                tt(POOL, ndbd[:, :], dec[:, :], C("nbd"), ALU.mult, [dec.r(), cr], [ndbd.r()])
                dlo = F128.next()
                tt(POOL, dlo[:, :], dec[:, :], C("lom"), ALU.mult, [dec.r(), cr], [dlo.r()])
                Pk = F128.next()
                stt(Pk[:, :], PSf(bS, 0, 128), sm[:, 0:1], ndbd[:, :], ALU.mult, ALU.mult, [rKK, sr, ndbd.r()], [Pk.r()])
                Lo = LO.next()
                stt(Lo[:, :], PSf(bS, 0, 128), sm[:, 0:1], dlo[:, :], ALU.mult, ALU.mult, [rKK, sr, dlo.r()], [Lo.r()])
                rb_, rc_, rd_ = PR(bS, 128, 256), PR(bS, 256, 384), PR(bS, 384, 512)
                tr(PSf(bS, 128, 256), Pk[:, :], IF, [Pk.r(), cr], [rb_])
                Mk = F128.next()
                cp(ACT, Mk[:, :], PSf(bS, 128, 256), [rb_], [Mk.r()])
                R = F128.next()
                tt(DVE, R[:, :], Mk[:, :], IF, ALU.add, [Mk.r(), cr], [R.r()])
                for k in range(5):
                    if k < 4:
                        mm(PSf(bS, 256, 384), Pk[:, :], Mk[:, :], True, True, [Pk.r(), Mk.r()], [rc_])
                    mm(PSf(bS, 384, 512), Mk[:, :], Pk[:, :], True, True, [Pk.r(), Mk.r()], [rd_])
                    Pn = F128.next()
                    cp(ACT, Pn[:, :], PSf(bS, 384, 512), [rd_], [Pn.r()])
                    if k < 4:
                        Mn = F128.next()
                        cp(DVE, Mn[:, :], PSf(bS, 256, 384), [rc_], [Mn.r()])
                    mm(PSf(bS, 0, 128), Pn[:, :], R[:, :], True, True, [Pn.r(), R.r()], [rKK])
                    Rn = F128.next()
                    tt(DVE, Rn[:, :], R[:, :], PSf(bS, 0, 128), ALU.add, [R.r(), rKK], [Rn.r()])
                    R = Rn
                    Pk = Pn
                    if k < 4:
                        Mk = Mn
                TdT = R
                tr(PSf(bS, 128, 256), TdT[:, :], IF, [TdT.r(), cr], [rb_])
                Td = F128.next()
                cp(ACT, Td[:, :], PSf(bS, 128, 256), [rb_], [Td.r()])
                mm(PSf(bS, 256, 384), Lo[:, :], TdT[:, :], True, True, [Lo.r(), TdT.r()], [rc_])
                X = F128.next()
                cp(DVE, X[:, :], PSf(bS, 256, 384), [rc_], [X.r()])
                mm(PSf(bS, 384, 512), Td[:, :], X[:, :], True, True, [Td.r(), X.r()], [rd_])
                TT_ = B128.next()
                tt(DVE, TT_[:, :], TdT[:, :], PSf(bS, 384, 512), ALU.subtract, [TdT.r(), rd_], [TT_.r()])
                ru, rws = PR(bP, 0, 256), PR(bP, 256, 512)
                mm(PSf(bP, 0, 256), TT_[:, :], vb[:, :], True, True, [TT_.r(), vb.r()], [ru])
                u = F256.next()
                cp(ACT, u[:, :], PSf(bP, 0, 256), [ru], [u.r()])
                kbg = B128.next()
                ts(DVE, kbg[:, :], kn[:, :], sm[:, 13:14], None, ALU.mult, None, [kn.r(), sr], [kbg.r()])
                rw_, rqk = PR(bR, 256, 384), PR(bR, 384, 512)
                mm(PSf(bR, 256, 384), kbg[:, :], TT_[:, :], True, True, [kbg.r(), TT_.r()], [rw_])
                wT = B128.next()
                cp(ACT, wT[:, :], PSf(bR, 256, 384), [rw_], [wT.r()])
                kg = B128.next()
                ts(DVE, kg[:, :], kn[:, :], sm[:, 12:13], None, ALU.mult, None, [kn.r(), sr], [kg.r()])
                mm(PSf(bR, 384, 512), knT[:, :], qnT[:, :], True, True, [knT.r(), qnT.r()], [rqk])
                AT = B128.next()
                tt(DVE, AT[:, :], PSf(bR, 384, 512), decT[:, :], ALU.mult, [rqk, decT.r()], [AT.r()])
                while chain_done[0] < tl:
                    coop.yield_()
                mm(PSf(bP, 256, 512), wT[:, :], Sb[:, :], True, True, [wT.r(), Sb.r()], [rws])
                vnew = B256.next()
                tt(DVE, vnew[:, :], u[:, :], PSf(bP, 256, 512), ALU.subtract, [u.r(), rws], [vnew.r()])
                rqs, roi, rkv = PR(bQ, 0, 256), PR(bQ, 256, 512), PR(bR, 0, 256)
                mm(PSf(bQ, 0, 256), qnT[:, :], Sb[:, :], True, True, [qnT.r(), Sb.r()], [rqs])
                t1 = F256.next()
                act(t1[:, :], PSf(bQ, 0, 256), AF.Identity, [rqs, sr], [t1.r()], scale=sm[:, 9:10])
                mm(PSf(bQ, 256, 512), AT[:, :], vnew[:, :], True, True, [AT.r(), vnew.r()], [roi])
                o = F256.next()
                tt(DVE, o[:, :], PSf(bQ, 256, 512), t1[:, :], ALU.add, [roi, t1.r()], [o.r()])
                mm(PSf(bR, 0, 256), kg[:, :], vnew[:, :], True, True, [kg.r(), vnew.r()], [rkv])
                stt(Sf, Sf, sm[:, 11:12], PSf(bR, 0, 256), ALU.mult, ALU.add, [sfr, sr, rkv], [sfr])
                cp(ACT, Sb[:, :], Sf, [sfr], [Sb.r()])
                chain_done[0] = tl + 1
                junk2 = F256.next()
                act(junk2[:, :], o[:, :], AF.Square, [o.r()], [junk2.r(), sr], accum=sm[:, 14:15])
                ts(DVE, sm[:, 15:16], sm[:, 14:15], 1.0 / 256.0, 1e-6, ALU.mult, ALU.add, [sr], [sr])
                pw(sm[:, 16:17], sm[:, 15:16], nhalf[:, 0:1], [sr, nhalf.r()], [sr])
                y = B256.next()
                stt(y[:, :], o[:, :], sm[:, 16:17], gate[:, :], ALU.mult, ALU.mult, [o.r(), sr, gate.r()], [y.r()])
                y_transposes(y, yT, tsl, bS)

            coop.run([(lambda tl=tl: tile_body(tl)) for tl in range(4)], depth=GDN_DEPTH)
            out_proj(slot, yT, blk, gm)

    def phase_final(row0):
        for blk in range(NBLK):
            rs = rstd_block(blk)
            for dc in range(8):
                o = F512.next()
                stt(o[:, :], xT[:, dc, BS(blk)], Pm("now", dc, dc + 1), rs[:, :], ALU.mult, ALU.mult,
                    [xr(dc, blk), pr_, rs.r()], [o.r()])
                bank = dc % 4
                for tl in range(4):
                    tr(PSf(bank, tl * 128, (tl + 1) * 128), o[:, tl * 128:(tl + 1) * 128], C("identF"), [o.r(), cr], [PR(bank)])
                ob = OB.next()
                q = ACT if dc % 2 == 0 else DVE
                cp(q, ob[:, :], PSf(bank), [PR(bank)], [ob.r()])
                r0 = row0 + blk * 512
                dma(SP, y_d[r0:r0 + 512, dc * 128:(dc + 1) * 128].rearrange("(t p) c -> p t c", p=128),
                    ob[:, :].rearrange("p (t c) -> p t c", t=4), [ob.r()], [], ob.name)

    for pi in range(n_pass):
        s, hf = pi // 2, pi % 2
        row0 = s * SEQ + hf * TOK
        pe_barrier()
        jobs = []
        if "mix_ret" in stages:
            jobs += list(range(4))
        if "mix_gdn" in stages:
            jobs += list(range(4, 8))
        if jobs:
            nxt = load_job(jobs[0])
        phase_load(row0)
        phase_norm_mix(s)
        if jobs:
            full_barrier()
            for ji, job in enumerate(jobs):
                slot = nxt
                if ji + 1 < len(jobs):
                    nxt = load_job(jobs[ji + 1])
                pe_barrier()
                if job < 4:
                    ret_job(s, hf, job, slot)
                else:
                    gdn_job(s, hf, job - 4, slot)
        if jobs:
            full_barrier()
        if "moe" in stages:
            pe_barrier()
            eslots = {0: load_expert(0), 1: load_expert(1)}
            phase_norm_ffn(s)
            pe_barrier()
            phase_moe(s, eslots)
        pe_barrier()
        phase_final(row0)
    for t_ in OB.tiles:
        od = dsem(t_.name)
        nc.sync.wait_ge(od.sem, od.n)
    return nc


def _host_pack(inp):
    f = lambda a: np.ascontiguousarray(np.asarray(a, dtype=np.float32))
    w_in = f(inp["w_in"])[0]
    w_out = f(inp["w_out"])[0]
    wj = np.zeros((8, DM, WCOLS), np.float32)
    wo = np.zeros((8, 256, DM), np.float32)
    for h in range(4):
        q = w_in[:, O_RQ + h * 128:O_RQ + (h + 1) * 128]
        k = w_in[:, O_RK + h * 128:O_RK + (h + 1) * 128]
        sw = lambda a: np.concatenate([a[:, 64:128], a[:, 0:64]], axis=1)
        wj[h, :, 0:128] = q
        wj[h, :, 128:256] = sw(q)
        wj[h, :, 256:384] = k
        wj[h, :, 384:512] = sw(k)
        wj[h, :, 512:768] = w_in[:, O_RV + h * 256:O_RV + (h + 1) * 256]
        wj[h, :, 768:1024] = w_in[:, O_RG + h * 256:O_RG + (h + 1) * 256]
        wj[h, :, 1024:1280] = w_in[:, O_MA + h * 256:O_MA + (h + 1) * 256]
        g = 4 + h
        wj[g, :, 0:128] = w_in[:, O_GQ + h * 128:O_GQ + (h + 1) * 128]
        wj[g, :, 128:256] = w_in[:, O_GK + h * 128:O_GK + (h + 1) * 128]
        wj[g, :, 256:512] = w_in[:, O_GV + h * 256:O_GV + (h + 1) * 256]
        wj[g, :, 512:768] = w_in[:, O_GZ + h * 256:O_GZ + (h + 1) * 256]
        wj[g, :, 768:1024] = w_in[:, O_MB + h * 256:O_MB + (h + 1) * 256]
        wj[g, :, 1024] = w_in[:, O_GA + h]
        wj[g, :, 1025] = w_in[:, O_GB + h]
        wo[h] = w_out[h * 256:(h + 1) * 256, :]
        wo[g] = w_out[h * 256:(h + 1) * 256, :]
    prm = np.zeros((128, PRM_N), np.float32)

    def put(name, arr):
        o, w = PRM_OFF[name]
        prm[:, o:o + w] = arr

    fm = lambda v: f(v).reshape(-1, 128).T
    put("nmw", fm(inp["norm_mix_w"][0]))
    put("nfw", fm(inp["norm_ffn_w"][0]))
    put("now", fm(inp["norm_out_w"]))
    put("modb", fm(inp["mod_b"][0]))
    cw = f(inp["gdn_conv_w"])[0]
    put("convw", cw.reshape(4, 16, 128).transpose(2, 1, 0).reshape(128, 64))
    put("gnw", np.broadcast_to(f(inp["gdn_norm_w"])[0][None, :], (128, 256)))
    put("alog", np.broadcast_to(f(inp["gdn_a_log"])[0][None, :], (128, 4)))
    put("dtb", np.broadcast_to(f(inp["gdn_dt_bias"])[0][None, :], (128, 4)))
    rb = np.concatenate([f(inp["b_group"])[0].reshape(-1), f(inp["b_router"])[0].reshape(-1)])
    put("rbias", np.broadcast_to(rb[None, :], (128, 20)))
    wr = np.concatenate([f(inp["w_group"])[0], f(inp["w_router"])[0]], axis=1)
    put("Wrt", wr.reshape(8, 128, 20).transpose(1, 0, 2).reshape(128, 160))
    cst_np, rope_np = make_consts()
    shared = {
        "cst": cst_np, "rope": rope_np,
        "modw": f(inp["mod_w"])[0],
        "wj": wj, "wo": wo,
        "wg": f(inp["w_gate"])[0].reshape(16, DM, 512),
        "wu": f(inp["w_up"])[0].reshape(16, DM, 512),
        "wd": f(inp["w_down"])[0].reshape(16, 512, DM),
    }
    x = f(inp["x"])
    c = f(inp["c"])
    in_maps = []
    for core in range(NCORES):
        p = prm.copy()
        o, w = PRM_OFF["cT"]
        cc = c[core * SPC:(core + 1) * SPC]
        p[:, o:o + w] = cc.T.reshape(8, 128, 2).transpose(1, 0, 2).reshape(128, 16)
        m = dict(shared)
        m["prm"] = p
        m["x"] = x[core * SPC:(core + 1) * SPC].reshape(SPC * SEQ, DM)
        in_maps.append(m)
    return in_maps


_NC_CACHE = {}


def kernel(**inputs):
    in_maps = _host_pack(inputs)
    if "nc" not in _NC_CACHE:
        _NC_CACHE["nc"] = build_program()
    nc = _NC_CACHE["nc"]
    res = run_bass_kernel_spmd(nc, in_maps, core_ids=list(range(NCORES)))
    out = np.stack([r["y"].reshape(SPC, SEQ, DM) for r in res.results], 0).reshape(NCORES * SPC, SEQ, DM)
    return np.ascontiguousarray(out.astype(np.float32))
```

```python
import threading
import numpy as np
import concourse.bass as bass
import concourse.mybir as mybir
from concourse.bass_utils import run_bass_kernel_spmd

F32 = mybir.dt.float32
BF16 = mybir.dt.bfloat16
AF = mybir.ActivationFunctionType
ALU = mybir.AluOpType
AX = mybir.AxisListType

SEQ = 2048
TOK = 1024
NBLK = TOK // 512
DM = 1024
NCORES = 8
SPC = 2
WCOLS = 1280
DK_SCALE = 128.0 ** -0.5

O_RQ, O_RK, O_RV, O_RG = 0, 512, 1024, 2048
O_GQ, O_GK, O_GV, O_GZ = 3072, 3584, 4096, 5120
O_GA, O_GB, O_MA, O_MB = 6144, 6148, 6152, 7176


def _cst_layout():
    names = [("identF", 128), ("ones", 128), ("nones", 128), ("Uc", 128), ("nUc", 128), ("negm", 128),
             ("negmT", 128), ("nbd", 128), ("lom", 128), ("DTr", 512), ("kdec", 4), ("epsr", 4),
             ]
    off = {}
    o = 0
    for n, w in names:
        off[n] = (o, w)
        o += w
    return off, o


CST_OFF, CST_N = _cst_layout()


def make_consts():
    c = np.zeros((128, CST_N), np.float32)

    def put(name, arr):
        o, w = CST_OFF[name]
        c[:arr.shape[0], o:o + w] = arr

    i = np.arange(128)
    put("identF", np.eye(128, dtype=np.float32))
    put("ones", np.ones((128, 128), np.float32))
    put("nones", -np.ones((128, 128), np.float32))
    Uc = (i[:, None] <= i[None, :]).astype(np.float32)
    put("Uc", Uc)
    put("nUc", -Uc)
    negm = np.where(i[None, :] <= i[:, None], 0.0, -30000.0).astype(np.float32)
    put("negm", negm)
    put("negmT", negm.T.copy())
    nbd = np.where((i[:, None] > i[None, :]) & ((i[:, None] // 64) == (i[None, :] // 64)), -1.0, 0.0)
    put("nbd", nbd.astype(np.float32))
    lom = np.where((i[:, None] >= 64) & (i[None, :] < 64), 1.0, 0.0)
    put("lom", lom.astype(np.float32))
    dtr = np.zeros((128, 512), np.float64)
    kdec = np.zeros((128, 4), np.float64)
    epsr = np.zeros((128, 4), np.float64)
    for h in range(4):
        gam = 1.0 - 2.0 ** (-5.0 - h)
        dtr[:, h * 128:(h + 1) * 128] = (gam ** (-(i[:, None] + 1.0))) * DK_SCALE * (i[None, :] >= i[:, None])
        kdec[:, h] = gam ** (127.0 - i) * DK_SCALE
        epsr[:, h] = 1e-6 * gam ** (-2.0 * (i + 1.0))
    put("DTr", dtr.astype(np.float32))
    put("kdec", kdec.astype(np.float32))
    put("epsr", epsr.astype(np.float32))
    half = 64
    inv_freq = (1.0 / (np.float32(10000.0) ** (np.arange(half, dtype=np.float32) / np.float32(half)))).astype(np.float32)
    ang = (np.arange(SEQ, dtype=np.float32)[:, None] * inv_freq[None, :]).astype(np.float32)
    cos = np.cos(ang).astype(np.float32).T
    sin = np.sin(ang).astype(np.float32).T
    rope = np.stack([np.concatenate([cos, cos], 0), np.concatenate([-sin, sin], 0)], 1)
    return c, np.ascontiguousarray(rope.astype(np.float32))


def _prm_layout():
    names = [("nmw", 8), ("nfw", 8), ("now", 8), ("modb", 48), ("convw", 64), ("gnw", 256), ("alog", 4),
             ("dtb", 4), ("rbias", 20), ("Wrt", 160), ("cT", 16)]
    off = {}
    o = 0
    for n, w in names:
        off[n] = (o, w)
        o += w
    return off, o


PRM_OFF, PRM_N = _prm_layout()


class Reg:
    __slots__ = ("w", "r", "excl")

    def __init__(self, excl=False):
        self.w = None
        self.r = {}
        self.excl = excl


class Q:
    def __init__(self, name, eng, sem, is_pe=False):
        self.name, self.eng, self.sem, self.n, self.seen, self.is_pe = name, eng, sem, 0, {}, is_pe


class DSem:
    def __init__(self, sem):
        self.sem, self.n = sem, 0


def emit(q, fn, reads=(), writes=(), dsem=None):
    need = {}
    ex = [r for r in reads if r.excl and r not in writes]
    if ex:
        writes = list(writes) + ex

    def add(tok):
        sem, val, src = tok
        if src is q and q.is_pe:
            return
        k = id(sem)
        if k not in need or need[k][1] < val:
            need[k] = (sem, val)

    for r in reads:
        if r.w is not None:
            add(r.w)
    for w in writes:
        if w.w is not None:
            add(w.w)
        for t in w.r.values():
            add(t)
    for k, (sem, val) in need.items():
        if q.seen.get(k, 0) >= val:
            continue
        q.eng.wait_ge(sem, val)
        q.seen[k] = val
    ins = fn()
    if dsem is None:
        q.n += 1
        ins.then_inc(q.sem, 1)
        tok = (q.sem, q.n, q)
    else:
        dsem.n += 16
        ins.then_inc(dsem.sem, 16)
        tok = (dsem.sem, dsem.n, None)
    for r in reads:
        k = id(tok[0])
        r.r[k] = tok
    for w in writes:
        w.w = tok
        w.r = {}
    return tok


class Coop:
    def __init__(self):
        self.local = threading.local()

    def yield_(self):
        t = getattr(self.local, "task", None)
        if t is None:
            return
        t.main.release()
        t.go.acquire()

    def run(self, fns, depth):
        if depth <= 1:
            for f in fns:
                f()
            return

        class T:
            pass

        pending = list(fns)
        active = []

        def start(fn):
            t = T()
            t.go = threading.Semaphore(0)
            t.main = threading.Semaphore(0)
            t.done = False
            t.exc = None

            def body():
                self.local.task = t
                t.go.acquire()
                try:
                    fn()
                except BaseException as ex:
                    t.exc = ex
                t.done = True
                t.main.release()

            t.th = threading.Thread(target=body, daemon=True)
            t.th.start()
            return t

        while pending or active:
            while len(active) < depth and pending:
                active.append(start(pending.pop(0)))
            for t in list(active):
                t.go.release()
                t.main.acquire()
                if t.exc is not None:
                    raise t.exc
                if t.done:
                    active.remove(t)


class Tile:
    def __init__(self, nc, name, shape, dtype, psum=False, at=None):
        if at is not None:
            self.t = nc.alloc_sbuf_tensor_at("t_" + name, shape, dtype, offset=at)
        else:
            self.t = (nc.alloc_psum_tensor if psum else nc.alloc_sbuf_tensor)("t_" + name, shape, dtype)
        self.name = name
        self.psum = psum
        self.regs = {}

    def r(self, key=None):
        if key not in self.regs:
            self.regs[key] = Reg(excl=self.psum)
        return self.regs[key]

    def __getitem__(self, idx):
        return self.t[idx]


class Arena:
    def __init__(self, off, size):
        self.off, self.size, self.cur = off, size, 0

    def take(self, nbytes):
        nbytes = (nbytes + 31) // 32 * 32
        assert self.cur + nbytes <= self.size, ("arena overflow", self.cur, nbytes, self.size)
        o = self.off + self.cur
        self.cur += nbytes
        return o


class Pool_:
    def __init__(self, nc, name, shape, dtype, n, arena=None, n_arena=0, esize=4):
        self.tiles = [Tile(nc, f"{name}{i}", shape, dtype) for i in range(n)]
        per = esize
        for s_ in shape[1:]:
            per *= s_
        for i in range(n_arena):
            self.tiles.append(Tile(nc, f"{name}x{i}", shape, dtype, at=arena.take(per)))
        self.i = 0

    def next(self):
        t = self.tiles[self.i % len(self.tiles)]
        self.i += 1
        return t


PS_GRAN = [512] * 8
GDN_DEPTH = 4
BANKSETS = {1: [(4, 5, 6, 7)] * 4, 2: [(4, 5, 6, 7), (0, 1, 2, 3)] * 2,
            4: [(2 * t, 2 * t + 1, 2 * t, 2 * t + 1) for t in range(4)]}


def build_program(stages=("mix_ret", "mix_gdn", "moe"), n_pass=2 * SPC):
    nc = bass.Bass("TRN2", target_bir_lowering=False)
    NTOK = SPC * SEQ
    x_d = nc.dram_tensor("x", [NTOK, DM], F32, kind="ExternalInput").ap()
    cst_d = nc.dram_tensor("cst", [128, CST_N], F32, kind="ExternalInput").ap()
    rope_d = nc.dram_tensor("rope", [128, 2, SEQ], F32, kind="ExternalInput").ap()
    prm_d = nc.dram_tensor("prm", [128, PRM_N], F32, kind="ExternalInput").ap()
    modw_d = nc.dram_tensor("modw", [DM, 6 * DM], F32, kind="ExternalInput").ap()
    wj_d = nc.dram_tensor("wj", [8, DM, WCOLS], F32, kind="ExternalInput").ap()
    wo_d = nc.dram_tensor("wo", [8, 256, DM], F32, kind="ExternalInput").ap()
    wg_d = nc.dram_tensor("wg", [16, DM, 512], F32, kind="ExternalInput").ap()
    wu_d = nc.dram_tensor("wu", [16, DM, 512], F32, kind="ExternalInput").ap()
    wd_d = nc.dram_tensor("wd", [16, 512, DM], F32, kind="ExternalInput").ap()
    y_d = nc.dram_tensor("y", [NTOK, DM], F32, kind="ExternalOutput").ap()

    PE = Q("pe", nc.tensor, nc.alloc_semaphore("s_pe"), is_pe=True)
    ACT = Q("act", nc.scalar, nc.alloc_semaphore("s_act"))
    DVE = Q("dve", nc.vector, nc.alloc_semaphore("s_dve"))
    POOL = Q("pool", nc.gpsimd, nc.alloc_semaphore("s_pool"))
    SP = Q("sp", nc.sync, nc.alloc_semaphore("s_sp"))
    dsem_cache = {}

    def dsem(name):
        if name not in dsem_cache:
            dsem_cache[name] = DSem(nc.alloc_semaphore("d_" + name))
        return dsem_cache[name]

    def flat(lst):
        out = []
        for a in lst:
            if isinstance(a, (list, tuple)):
                out.extend(flat(a))
            else:
                out.append(a)
        return out

    coop = Coop()

    def E(q, fn, reads, writes, dsem_=None):
        tok = emit(q, fn, flat(reads), flat(writes), dsem=dsem_)
        coop.yield_()
        return tok

    def dma(q, out, in_, reads, writes, sem):
        return E(q, lambda: q.eng.dma_start(out=out, in_=in_), reads, writes, dsem(sem))

    def mm(out, lhsT, rhs, start, stop, reads, writes):
        return E(PE, lambda: nc.tensor.matmul(out, lhsT, rhs, start=start, stop=stop), reads, writes)

    def tr(out, in_, ident, reads, writes):
        return E(PE, lambda: nc.tensor.transpose(out, in_, ident), reads, writes)

    def act(out, in_, func, reads, writes, bias=None, scale=None, accum=None):
        kw = {}
        if bias is not None:
            kw["bias"] = bias
        if scale is not None:
            kw["scale"] = scale
        if accum is not None:
            kw["accum_out"] = accum
        return E(ACT, lambda: nc.scalar.activation(out=out, in_=in_, func=func, **kw), reads, writes)

    def tt(q, out, in0, in1, op, reads, writes):
        return E(q, lambda: q.eng.tensor_tensor(out=out, in0=in0, in1=in1, op=op), reads, writes)

    def ts(q, out, in0, s1, s2, op0, op1, reads, writes):
        if s2 is None:
            return E(q, lambda: q.eng.tensor_scalar(out=out, in0=in0, scalar1=s1, scalar2=None, op0=op0), reads, writes)
        return E(q, lambda: q.eng.tensor_scalar(out=out, in0=in0, scalar1=s1, scalar2=s2, op0=op0, op1=op1), reads, writes)

    def stt(out, in0, scalar, in1, op0, op1, reads, writes, accum=None):
        if accum is not None:
            return E(DVE, lambda: nc.vector.scalar_tensor_tensor(out=out, in0=in0, scalar=scalar, in1=in1, op0=op0, op1=op1, accum_out=accum), reads, writes)
        return E(DVE, lambda: nc.vector.scalar_tensor_tensor(out=out, in0=in0, scalar=scalar, in1=in1, op0=op0, op1=op1), reads, writes)

    def cp(q, out, in_, reads, writes):
        if q is ACT:
            return E(q, lambda: nc.scalar.activation(out=out, in_=in_, func=AF.Copy), reads, writes)
        return E(q, lambda: q.eng.tensor_copy(out=out, in_=in_), reads, writes)

    def memset(q, ap, val, writes):
        return E(q, lambda: q.eng.memset(ap, val), [], writes)

    def pw(out, in0, in1, reads, writes):
        return E(POOL, lambda: nc.gpsimd.tensor_tensor(out=out, in0=in0, in1=in1, op=ALU.pow), reads, writes)

    cst = Tile(nc, "cst", [128, CST_N], F32)
    prm = Tile(nc, "prm", [128, PRM_N], F32)
    cr, pr_ = cst.r(), prm.r()

    def C(name, lo=0, hi=None, rows=128):
        o, w = CST_OFF[name]
        hi = w if hi is None else hi
        return cst[0:rows, o + lo:o + hi]

    def Pm(name, lo=0, hi=None):
        o, w = PRM_OFF[name]
        hi = w if hi is None else hi
        return prm[:, o + lo:o + hi]

    identB = Tile(nc, "identB", [128, 128], BF16)
    nhalf = Tile(nc, "nhalf", [128, 8], F32)
    xT = Tile(nc, "xT", [128, 8, TOK], F32)
    xmT = Tile(nc, "xmT", [128, 8, TOK], BF16)
    WA = [Tile(nc, f"WA{i}", [128, 6144], F32) for i in range(2)]
    combT = Tile(nc, "combT", [16, TOK], F32)
    misc = Tile(nc, "misc", [128, 256], F32)
    SfA = Tile(nc, "SfA", [128, 8, 256], F32)
    HAL = Tile(nc, "HAL", [128, 16, 4], F32)
    Sb = Tile(nc, "Sb", [128, 256], BF16)
    PSt = [Tile(nc, f"ps{i}", [128, 512], F32, psum=True) for i in range(8)]

    def PR(bank, c0=0, c1=512):
        g = PS_GRAN[bank]
        return [PSt[bank].r(k) for k in range(c0 // g, (c1 - 1) // g + 1)]

    def PSf(bank, c0=0, c1=512, rows=128):
        return PSt[bank][0:rows, c0:c1]

    def psb(bank, c0, c1):
        return PSt[bank].t[:].bitcast(BF16)[:, 2 * c0:2 * c1]

    def WAf(slot):
        return WA[slot].t[:].rearrange("p (k n) -> p k n", k=8)

    def WAb(slot):
        return WA[slot].t[:].bitcast(BF16)

    ARENA_BYTES = 22528
    _base = (nc.sbuf_base + 31) // 32 * 32
    _slab = nc.alloc_sbuf_tensor("t_arena", [128, ARENA_BYTES // 4], F32)
    arA = Arena(_base, ARENA_BYTES)
    arB = Arena(_base, ARENA_BYTES)
    XIN = Pool_(nc, "xin", [128, 1024], F32, 0, arena=arA, n_arena=2)
    ROPE = Pool_(nc, "rope", [128, 2, 512], F32, 1)
    F512 = Pool_(nc, "f512", [128, 512], F32, 4)
    OB = Pool_(nc, "ob", [128, 512], F32, 0, arena=arA, n_arena=2)
    RS = Pool_(nc, "rs", [128, 512], F32, 2)
    CBS = Pool_(nc, "cbs", [128, 512], F32, 0, arena=arA, n_arena=1)
    DECT = Pool_(nc, "dect", [128, 128], F32, 3, arena=arB, n_arena=2)
    LO = Pool_(nc, "lo", [128, 128], F32, 3, arena=arB, n_arena=2)
    B512 = Pool_(nc, "b512", [128, 512], BF16, 6)
    F256 = Pool_(nc, "f256", [128, 256], F32, 8, arena=arB, n_arena=6)
    B256 = Pool_(nc, "b256", [128, 256], BF16, 8, arena=arB, n_arena=4, esize=2)
    F128 = Pool_(nc, "f128", [128, 128], F32, 16, arena=arB, n_arena=12)
    B128 = Pool_(nc, "b128", [128, 128], BF16, 20, arena=arB, n_arena=16, esize=2)
    GATE = Pool_(nc, "gate", [128, 256], F32, 3, arena=arB, n_arena=2)
    SM = Pool_(nc, "sm", [128, 32], F32, 8)
    YT = Pool_(nc, "yT", [128, 2, 512], BF16, 2)
    ACTT = Pool_(nc, "actT", [128, 4, 512], BF16, 0, arena=arA, n_arena=2, esize=2)
    CB = [Tile(nc, f"cb{c}", [128, 516], F32) for c in range(4)]

    def full_barrier():
        qs = (PE, ACT, DVE, POOL, SP)
        targets = [(p.sem, p.n, p) for p in qs] + [(d_.sem, d_.n, None) for d_ in dsem_cache.values()]
        for q in qs:
            for (sem, val, p) in targets:
                if p is q or val == 0:
                    continue
                if val > q.seen.get(id(sem), 0):
                    q.eng.wait_ge(sem, val)
                    q.seen[id(sem)] = val

    def pe_barrier():
        for q in (ACT, DVE, POOL):
            if q.n > PE.seen.get(id(q.sem), 0):
                nc.tensor.wait_ge(q.sem, q.n)
                PE.seen[id(q.sem)] = q.n

    dma(SP, cst[:, :], cst_d[:, :], [], [cr], "cst")
    dma(SP, prm[:, :], prm_d[:, :], [], [pr_], "prm")
    cp(DVE, identB[:, :], C("identF"), [cr], [identB.r()])
    memset(DVE, nhalf[:, :], -0.5, [nhalf.r()])
    mr = misc.r()
    scT = misc[:, 0:16]
    act(scT, Pm("cT"), AF.Silu, [pr_], [mr])
    act(misc[:, 16:20], Pm("alog"), AF.Exp, [pr_], [mr])
    ts(DVE, misc[:, 20:24], misc[:, 16:20], -1.0, None, ALU.mult, None, [mr], [mr])
    NEA = misc[:, 20:24]
    memset(DVE, misc[:, 24:25], 1e-6, [mr])
    memset(DVE, misc[:, 25:26], 128.0e-6, [mr])
    EPSB = misc[:, 24:26]

    modw_v = modw_d.rearrange("(kc p) n -> p kc n", p=128)
    wslot = [0]
    for cbk in range(8):
        slot = wslot[0] % 2
        wslot[0] += 1
        dma(SP, WAf(slot), modw_v[:, :, cbk * 768:(cbk + 1) * 768], [], [WA[slot].r()], f"mw{slot}")
        for j in range(6):
            jb = cbk * 6 + j
            for kc in range(8):
                mm(PSf(0, jb * 2, jb * 2 + 2), WAf(slot)[:, kc, j * 128:(j + 1) * 128], scT[:, kc * 2:kc * 2 + 2],
                   kc == 0, kc == 7, [WA[slot].r(), mr], [PR(0)])
    ps3 = PSf(0, 0, 96).rearrange("p (j b) -> p j b", b=2)
    for b in range(2):
        tt(DVE, misc[:, 32 + 48 * b:80 + 48 * b], ps3[:, :, b], Pm("modb"), ALU.add, [PR(0), pr_], [mr])

    def modv(b, k):
        return misc[:, 32 + 48 * b + 8 * k:32 + 48 * b + 8 * k + 8]

    for b in range(2):
        for (dst, k, nw) in ((128 + 8 * b, 1, "nmw"), (144 + 8 * b, 4, "nfw")):
            ts(DVE, misc[:, dst:dst + 8], modv(b, k), 1.0, None, ALU.add, None, [mr], [mr])
            tt(DVE, misc[:, dst:dst + 8], misc[:, dst:dst + 8], Pm(nw), ALU.mult, [mr, pr_], [mr])

    def A1(b):
        return misc[:, 128 + 8 * b:136 + 8 * b]

    def A2(b):
        return misc[:, 144 + 8 * b:152 + 8 * b]

    def xr(dc, blk):
        return xT.r((dc, blk))

    def xmr(dc, blk):
        return xmT.r((dc, blk))

    def BS(blk):
        return slice(blk * 512, (blk + 1) * 512)

    def phase_load(row0):
        for t in range(TOK // 128):
            xin = XIN.next()
            dma(SP, xin[:, :], x_d[row0 + t * 128:row0 + (t + 1) * 128, :], [], [xin.r()], xin.name)
            for half in range(2):
                bank = (2 * t + half) % 4
                for j in range(4):
                    dc = half * 4 + j
                    tr(PSf(bank, j * 128, (j + 1) * 128), xin[:, dc * 128:(dc + 1) * 128], C("identF"),
                       [xin.r(), cr], [PR(bank)])
                q = ACT if half == 0 else DVE
                cp(q, xT[:, half * 4:half * 4 + 4, t * 128:(t + 1) * 128],
                   PSf(bank).rearrange("p (j c) -> p j c", j=4), [PR(bank)],
                   [xr(half * 4 + j, t // 4) for j in range(4)])

    def rstd_block(blk):
        for dc in range(8):
            sq = F512.next()
            act(sq[:, :], xT[:, dc, BS(blk)], AF.Square, [xr(dc, blk)], [sq.r()])
            mm(PSf(4), C("ones"), sq[:, :], dc == 0, dc == 7, [cr, sq.r()], [PR(4)])
        t1 = F512.next()
        act(t1[:, :], PSf(4), AF.Ln, [PR(4), mr], [t1.r()], bias=EPSB[:, 0:1], scale=1.0 / DM)
        rs = RS.next()
        act(rs[:, :], t1[:, :], AF.Exp, [t1.r()], [rs.r()], scale=-0.5)
        return rs

    def phase_norm_mix(b):
        for blk in range(NBLK):
            rs = rstd_block(blk)
            for dc in range(8):
                tmp = F512.next()
                stt(tmp[:, :], xT[:, dc, BS(blk)], A1(b)[:, dc:dc + 1], rs[:, :], ALU.mult, ALU.mult,
                    [xr(dc, blk), mr, rs.r()], [tmp.r()])
                act(xmT[:, dc, BS(blk)], tmp[:, :], AF.Identity, [tmp.r(), mr], [xmr(dc, blk)],
                    bias=modv(b, 0)[:, dc:dc + 1])

    def routing(tlg, bank):
        l = SM.next()
        lr = l.r()
        tt(DVE, l[:, 0:20], PSf(bank, 0, 20), Pm("rbias"), ALU.add, [PR(bank, 0, 20), pr_], [lr])
        w = SM.next()
        wr = w.r()
        E(DVE, lambda: nc.vector.tensor_reduce(out=w[:, 0:1], in_=l[:, 0:4], axis=AX.X, op=ALU.max), [lr], [wr])
        ts(DVE, w[:, 1:2], w[:, 0:1], -1.0, None, ALU.mult, None, [wr], [wr])
        act(w[:, 22:26], l[:, 0:4], AF.Exp, [lr, wr], [wr], bias=w[:, 1:2], accum=w[:, 2:3])
        E(DVE, lambda: nc.vector.reciprocal(out=w[:, 3:4], in_=w[:, 2:3]), [wr], [wr])
        ts(DVE, w[:, 4:8], l[:, 0:4], w[:, 0:1], None, ALU.is_equal, None, [lr, wr], [wr])
        ts(DVE, w[:, 8:12], l[:, 4:8], w[:, 4:5], None, ALU.mult, None, [lr, wr], [wr])
        for g in range(1, 4):
            stt(w[:, 8:12], l[:, 4 + 4 * g:8 + 4 * g], w[:, 4 + g:5 + g], w[:, 8:12], ALU.mult, ALU.add, [lr, wr], [wr])
        E(DVE, lambda: nc.vector.tensor_reduce(out=w[:, 12:13], in_=w[:, 8:12], axis=AX.X, op=ALU.max), [wr], [wr])
        ts(DVE, w[:, 13:14], w[:, 12:13], -1.0, None, ALU.mult, None, [wr], [wr])
        ts(DVE, w[:, 14:18], w[:, 8:12], w[:, 12:13], None, ALU.is_equal, None, [wr], [wr])
        stt(w[:, 14:18], w[:, 14:18], -1e30, w[:, 8:12], ALU.mult, ALU.add, [wr], [wr])
        E(DVE, lambda: nc.vector.tensor_reduce(out=w[:, 18:19], in_=w[:, 14:18], axis=AX.X, op=ALU.max), [wr], [wr])
        ts(DVE, w[:, 14:18], w[:, 8:12], w[:, 18:19], None, ALU.is_ge, None, [wr], [wr])
        act(w[:, 22:26], w[:, 8:12], AF.Exp, [wr], [wr], bias=w[:, 13:14])
        stt(w[:, 26:30], w[:, 22:26], 1.0, w[:, 14:18], ALU.mult, ALU.mult, [wr], [wr], accum=w[:, 19:20])
        E(DVE, lambda: nc.vector.reciprocal(out=w[:, 20:21], in_=w[:, 19:20]), [wr], [wr])
        tt(DVE, w[:, 21:22], w[:, 3:4], w[:, 20:21], ALU.mult, [wr], [wr])
        cm = SM.next()
        for g in range(4):
            ts(DVE, cm[:, 4 * g:4 * g + 4], w[:, 26:30], w[:, 4 + g:5 + g], w[:, 21:22], ALU.mult, ALU.mult, [wr], [cm.r()])
        tr(PSf(5, 0, 128, rows=16), cm[:, 0:16], C("identF"), [cm.r(), cr], [PR(5, 0, 128)])
        cp(ACT, combT[0:16, tlg * 128:(tlg + 1) * 128], PSf(5, 0, 128, rows=16), [PR(5, 0, 128)], [combT.r(tlg // 4)])

    def phase_norm_ffn(b):
        WrtF = Pm("Wrt").rearrange("p (k n) -> p k n", k=8)
        for blk in range(NBLK):
            rs = rstd_block(blk)
            for dc in range(8):
                tmp = F512.next()
                stt(tmp[:, :], xT[:, dc, BS(blk)], A2(b)[:, dc:dc + 1], rs[:, :], ALU.mult, ALU.mult,
                    [xr(dc, blk), mr, rs.r()], [tmp.r()])
                t2 = F512.next()
                act(t2[:, :], tmp[:, :], AF.Identity, [tmp.r(), mr], [t2.r()], bias=modv(b, 3)[:, dc:dc + 1])
                cp(DVE, xmT[:, dc, BS(blk)], t2[:, :], [t2.r()], [xmr(dc, blk)])
                for tl in range(4):
                    mm(PSf(tl, 0, 20), t2[:, tl * 128:(tl + 1) * 128], WrtF[:, dc, :], dc == 0, dc == 7,
                       [t2.r(), pr_], [PR(tl, 0, 20)])
            for tl in range(4):
                routing(blk * 4 + tl, tl)

    def load_expert(e):
        slot = wslot[0] % 2
        wslot[0] += 1
        wb = WAb(slot)
        rg = WA[slot].r()
        dma(POOL, wb[:, 0:4096].rearrange("p (k n) -> p k n", k=8), wg_d[e].rearrange("(kc p) f -> p kc f", p=128), [], [rg], f"wa{slot}")
        dma(POOL, wb[:, 4096:8192].rearrange("p (k n) -> p k n", k=8), wu_d[e].rearrange("(kc p) f -> p kc f", p=128), [], [rg], f"wa{slot}")
        dma(POOL, wb[:, 8192:12288].rearrange("p (k n) -> p k n", k=4), wd_d[e].rearrange("(fc p) d -> p fc d", p=128), [], [rg], f"wa{slot}")
        return slot

    def phase_moe(b):
        gf = modv(b, 5)
        slots = {0: load_expert(0), 1: load_expert(1)}

        def views(e):
            wb = WAb(slots[e])
            return (WA[slots[e]].r(), wb[:, 0:4096].rearrange("p (k n) -> p k n", k=8),
                    wb[:, 4096:8192].rearrange("p (k n) -> p k n", k=8),
                    wb[:, 8192:12288].rearrange("p (k n) -> p k n", k=4))

        def gate_up(e, blk):
            rg, Wg, Wu, Wd = views(e)
            bs = BS(blk)
            cme = F512.next()
            ts(DVE, cme[0:16, :], combT[0:16, bs], C("identF", e, e + 1, rows=16), None, ALU.mult, None,
               [combT.r(blk), cr], [cme.r()])
            aT = ACTT.next()
            cbs = None
            for fc in range(4):
                pg, pu = 2 * (fc % 2), 2 * (fc % 2) + 1
                for dc in range(8):
                    mm(PSf(pg), Wg[:, dc, fc * 128:(fc + 1) * 128], xmT[:, dc, bs], dc == 0, dc == 7, [rg, xmr(dc, blk)], [PR(pg)])
                for dc in range(8):
                    mm(PSf(pu), Wu[:, dc, fc * 128:(fc + 1) * 128], xmT[:, dc, bs], dc == 0, dc == 7, [rg, xmr(dc, blk)], [PR(pu)])
                if fc == 0:
                    mm(PSf(6), C("ones", rows=16), cme[0:16, :], True, True, [cr, cme.r()], [PR(6)])
                    cbs = CBS.next()
                    cp(ACT, cbs[:, :], PSf(6), [PR(6)], [cbs.r()])
                sg = F512.next()
                act(sg[:, :], PSf(pg), AF.Silu, [PR(pg)], [sg.r()])
                t = F512.next()
                tt(DVE, t[:, :], PSf(pu), sg[:, :], ALU.mult, [PR(pu), sg.r()], [t.r()])
                tt(POOL, aT[:, fc, :], t[:, :], cbs[:, :], ALU.mult, [t.r(), cbs.r()], [aT.r(fc)])
            return aT

        def down(e, blk, aT):
            rg, Wg, Wu, Wd = views(e)
            bs = BS(blk)
            for dc in range(8):
                po = (4, 5, 7)[dc % 3]
                for fc in range(4):
                    mm(PSf(po), Wd[:, fc, dc * 128:(dc + 1) * 128], aT[:, fc, :], fc == 0, fc == 3, [rg, aT.r(fc)], [PR(po)])
                stt(xT[:, dc, bs], PSf(po), gf[:, dc:dc + 1], xT[:, dc, bs], ALU.mult, ALU.add, [PR(po), mr, xr(dc, blk)], [xr(dc, blk)])

        units = [(e, blk) for e in range(16) for blk in range(NBLK)]
        prev = None
        for (e, blk) in units:
            aT = gate_up(e, blk)
            if prev is not None:
                pe_, pblk, paT = prev
                down(pe_, pblk, paT)
                if pblk == NBLK - 1 and pe_ + 2 < 16:
                    slots[pe_ + 2] = load_expert(pe_ + 2)
            prev = (e, blk, aT)
        down(*prev)

    def load_job(job):
        ncols = WCOLS if job < 4 else 1028
        slot = wslot[0] % 2
        wslot[0] += 1
        wb = WAb(slot)
        rg = WA[slot].r()
        dma(POOL, wb[:, 0:10240].rearrange("p (k n) -> p k n", k=8)[:, :, 0:ncols],
            wj_d[job].rearrange("(kc p) n -> p kc n", p=128)[:, :, 0:ncols], [], [rg], f"wa{slot}")
        dma(POOL, wb[:, 10240:12288].rearrange("p (k n) -> p k n", k=2), wo_d[job].rearrange("(cc p) d -> p cc d", p=128), [], [rg], f"wa{slot}")
        return slot

    def out_proj(slot, yT, blk, gm):
        rg = WA[slot].r()
        Wo = WAb(slot)[:, 10240:12288].rearrange("p (k n) -> p k n", k=2)
        for dc in range(8):
            po = dc % 4
            for cc in range(2):
                mm(PSf(po), Wo[:, cc, dc * 128:(dc + 1) * 128], yT[:, cc, :], cc == 0, cc == 1, [rg, yT.r()], [PR(po)])
            stt(xT[:, dc, BS(blk)], PSf(po), gm[:, dc:dc + 1], xT[:, dc, BS(blk)], ALU.mult, ALU.add, [PR(po), mr, xr(dc, blk)], [xr(dc, blk)])

    def y_transposes(y, yT, tsl, bk=7):
        for cc in range(2):
            tr(psb(bk, 64 * cc, 64 * cc + 64), y[:, cc * 128:(cc + 1) * 128], identB[:, :], [y.r(), identB.r()], [PR(bk, 0, 128)])
        cp(ACT, yT[:, 0:2, tsl], psb(bk, 0, 128).rearrange("p (c t) -> p c t", c=2), [PR(bk, 0, 128)], [yT.r()])

    def state_init(j, hf):
        sr_ = SfA.r(j)
        if hf == 0:
            memset(DVE, SfA[:, j, :], 0.0, [sr_])
        cp(ACT, Sb[:, :], SfA[:, j, :], [sr_], [Sb.r()])

    def ret_job(b, hf, h, slot):
        gm = modv(b, 2)
        rg = WA[slot].r()
        Wr = WAb(slot)[:, 0:10240].rearrange("p (k n) -> p k n", k=8)
        state_init(h, hf)
        Sf = SfA[:, h, :]
        sfr = SfA.r(h)
        gam128 = float((1.0 - 2.0 ** (-5.0 - h)) ** 128)
        for blk in range(NBLK):
            bs = BS(blk)
            pos0 = hf * TOK + blk * 512
            rp = ROPE.next()
            dma(SP, rp[:, :, :], rope_d[:, :, pos0:pos0 + 512], [], [rp.r()], "rope")
            for i in range(4):
                for dc in range(8):
                    mm(PSf(i), Wr[:, dc, i * 128:(i + 1) * 128], xmT[:, dc, bs], dc == 0, dc == 7, [rg, xmr(dc, blk)], [PR(i)])
            rot = []
            for (pa, pb_) in ((0, 1), (2, 3)):
                t1 = F512.next()
                tt(DVE, t1[:, :], PSf(pa), rp[:, 0, :], ALU.mult, [PR(pa), rp.r()], [t1.r()])
                t2 = F512.next()
                tt(DVE, t2[:, :], PSf(pb_), rp[:, 1, :], ALU.mult, [PR(pb_), rp.r()], [t2.r()])
                o = B512.next()
                tt(POOL, o[:, :], t1[:, :], t2[:, :], ALU.add, [t1.r(), t2.r()], [o.r()])
                rot.append(o)
            qrT, krT = rot
            yT = YT.next()
            chain_done = [0]

            def tile_body(tl):
                bP, bQ, bR, bS = BANKSETS[GDN_DEPTH][tl]
                tsl = slice(tl * 128, (tl + 1) * 128)
                gs = slice(blk * 512 + tl * 128, blk * 512 + (tl + 1) * 128)
                for dc in range(8):
                    mm(PSf(bP), xmT[:, dc, gs], Wr[:, dc, 512:1024], dc == 0, dc == 7, [rg, xmr(dc, blk)], [PR(bP)])
                for dc in range(8):
                    mm(PSf(bQ, 0, 256), xmT[:, dc, gs], Wr[:, dc, 1024:1280], dc == 0, dc == 7, [rg, xmr(dc, blk)], [PR(bQ, 0, 256)])
                vb = B256.next()
                cp(ACT, vb[:, :], PSf(bP, 0, 256), [PR(bP)], [vb.r()])
                sg = F256.next()
                act(sg[:, :], PSf(bP, 256, 512), AF.Silu, [PR(bP)], [sg.r()])
                sa = F256.next()
                act(sa[:, :], PSf(bQ, 0, 256), AF.Sigmoid, [PR(bQ, 0, 256)], [sa.r()])
                gate = GATE.next()
                tt(POOL, gate[:, :], sg[:, :], sa[:, :], ALU.mult, [sg.r(), sa.r()], [gate.r()])
                tr(psb(bQ, 256, 320), krT[:, tsl], identB[:, :], [krT.r(), identB.r()], [PR(bQ, 256, 320)])
                kin = B128.next()
                ts(DVE, kin[:, :], psb(bQ, 256, 320), C("kdec", h, h + 1), None, ALU.mult, None, [PR(bQ, 256, 320), cr], [kin.r()])
                mm(PSf(bQ, 384, 512), krT[:, tsl], qrT[:, tsl], True, True, [krT.r(), qrT.r()], [PR(bQ, 384, 512)])
                sT = B128.next()
                tt(DVE, sT[:, :], PSf(bQ, 384, 512), C("DTr", h * 128, (h + 1) * 128), ALU.mult, [PR(bQ, 384, 512), cr], [sT.r()])
                mm(PSf(bR, 0, 256), sT[:, :], vb[:, :], True, False, [sT.r(), vb.r()], [PR(bR, 0, 256)])
                while chain_done[0] < tl:
                    coop.yield_()
                mm(PSf(bR, 0, 256), qrT[:, tsl], Sb[:, :], False, True, [qrT.r(), Sb.r()], [PR(bR, 0, 256)])
                sm = SM.next()
                junk = F256.next()
                act(junk[:, :], PSf(bR, 0, 256), AF.Square, [PR(bR, 0, 256)], [junk.r(), sm.r()], accum=sm[:, 0:1])
                ts(DVE, sm[:, 1:2], sm[:, 0:1], 1.0 / 256.0, C("epsr", h, h + 1), ALU.mult, ALU.add, [sm.r(), cr], [sm.r()])
                pw(sm[:, 2:3], sm[:, 1:2], nhalf[:, 0:1], [sm.r(), nhalf.r()], [sm.r()])
                y = B256.next()
                stt(y[:, :], PSf(bR, 0, 256), sm[:, 2:3], gate[:, :], ALU.mult, ALU.mult, [PR(bR, 0, 256), sm.r(), gate.r()], [y.r()])
                y_transposes(y, yT, tsl, bS)
                mm(PSf(bR, 256, 512), kin[:, :], vb[:, :], True, True, [kin.r(), vb.r()], [PR(bR, 256, 512)])
                stt(Sf, Sf, gam128, PSf(bR, 256, 512), ALU.mult, ALU.add, [sfr, PR(bR, 256, 512)], [sfr])
                cp(ACT, Sb[:, :], Sf, [sfr], [Sb.r()])
                chain_done[0] = tl + 1

            coop.run([(lambda tl=tl: tile_body(tl)) for tl in range(4)], depth=GDN_DEPTH)
            out_proj(slot, yT, blk, gm)

    def gdn_job(b, hf, h, slot):
        gm = modv(b, 2)
        rg = WA[slot].r()
        Wg = WAb(slot)[:, 0:10240].rearrange("p (k n) -> p k n", k=8)
        j = 4 + h
        state_init(j, hf)
        Sf = SfA[:, j, :]
        sfr = SfA.r(j)
        hr = HAL.r(h)
        if hf == 0:
            memset(DVE, HAL[:, 4 * h:4 * h + 4, :], 0.0, [hr])
        chunks = [h, 4 + h, 8 + 2 * h, 9 + 2 * h]
        cw = Pm("convw")
        IF = C("identF")
        for blk in range(NBLK):
            bs = BS(blk)
            pT = []
            for cc in range(4):
                for dc in range(8):
                    mm(PSf(cc), Wg[:, dc, cc * 128:(cc + 1) * 128], xmT[:, dc, bs], dc == 0, dc == 7, [rg, xmr(dc, blk)], [PR(cc)])
                cb = CB[cc]
                cbr = cb.r()
                cp(DVE, cb[:, 0:3], HAL[:, 4 * h + cc, 0:3], [hr], [cbr])
                cp(ACT, cb[:, 3:515], PSf(cc), [PR(cc)], [cbr])
                cp(DVE, HAL[:, 4 * h + cc, 0:3], cb[:, 512:515], [cbr], [hr])
                acc = F512.next()
                ch = chunks[cc]
                ts(DVE, acc[:, :], cb[:, 0:512], cw[:, ch * 4:ch * 4 + 1], None, ALU.mult, None, [cbr, pr_], [acc.r()])
                for k in range(1, 4):
                    stt(acc[:, :], cb[:, k:k + 512], cw[:, ch * 4 + k:ch * 4 + k + 1], acc[:, :], ALU.mult, ALU.add, [cbr, pr_, acc.r()], [acc.r()])
                p = B512.next()
                act(p[:, :], acc[:, :], AF.Silu, [acc.r()], [p.r()])
                pT.append(p)
            yT = YT.next()
            chain_done = [0]

            def tile_body(tl):
                bP, bQ, bR, bS = BANKSETS[GDN_DEPTH][tl]
                tsl = slice(tl * 128, (tl + 1) * 128)
                gs = slice(blk * 512 + tl * 128, blk * 512 + (tl + 1) * 128)
                for dc in range(8):
                    mm(PSf(bP), xmT[:, dc, gs], Wg[:, dc, 512:1024], dc == 0, dc == 7, [rg, xmr(dc, blk)], [PR(bP)])
                rab = PR(bQ, 0, 64)
                for dc in range(8):
                    mm(PSf(bQ, 0, 2), xmT[:, dc, gs], Wg[:, dc, 1024:1026], dc == 0, dc == 7, [rg, xmr(dc, blk)], [rab])
                sm = SM.next()
                sr = sm.r()
                act(sm[:, 18:19], PSf(bQ, 1, 2), AF.Exp, [rab], [sr], scale=-1.0)
                ts(DVE, sm[:, 18:19], sm[:, 18:19], 1.0, None, ALU.add, None, [sr], [sr])
                E(DVE, lambda: nc.vector.reciprocal(out=sm[:, 0:1], in_=sm[:, 18:19]), [sr], [sr])
                act(sm[:, 1:2], PSf(bQ, 0, 1), AF.Exp, [rab, pr_], [sr], bias=Pm("dtb", h, h + 1))
                act(sm[:, 2:3], sm[:, 1:2], AF.Ln, [sr], [sr], bias=1.0)
                ts(DVE, sm[:, 3:4], sm[:, 2:3], NEA[:, h:h + 1], None, ALU.mult, None, [sr, mr], [sr])
                ts(DVE, sm[:, 4:5], sm[:, 2:3], NEA[:, h:h + 1], None, ALU.mult, None, [sr, mr], [sr])
                ee = F512.next()
                act(ee[:, :], PSf(bP), AF.Exp, [PR(bP)], [ee.r()], scale=-1.0)
                ts(DVE, ee[:, 256:512], ee[:, 256:512], 1.0, None, ALU.add, None, [ee.r()], [ee.r()])
                stt(ee[:, 0:256], ee[:, 0:256], 1.0, ee[:, 256:512], ALU.add, ALU.mult, [ee.r()], [ee.r()])
                sz = F256.next()
                E(DVE, lambda: nc.vector.reciprocal(out=sz[:, :], in_=ee[:, 0:256]), [ee.r()], [sz.r()])
                g3 = F256.next()
                tt(DVE, g3[:, :], PSf(bP, 0, 256), Pm("gnw"), ALU.mult, [PR(bP), pr_], [g3.r()])
                gate = GATE.next()
                tt(POOL, gate[:, :], g3[:, :], sz[:, :], ALU.mult, [g3.r(), sz.r()], [gate.r()])
                res = []
                for (idx, c_ssq, c_r, mulv, addv) in ((0, 5, 6, 128.0, 128.0e-6), (1, 7, 8, 1.0, 1e-6)):
                    ca = 64 + 64 * idx
                    ra = PR(bQ, ca, ca + 64)
                    pa = psb(bQ, ca, ca + 64)
                    tr(pa, pT[idx][:, tsl], identB[:, :], [pT[idx].r(), identB.r()], [ra])
                    junk = F128.next()
                    act(junk[:, :], pa, AF.Square, [ra], [junk.r(), sr], accum=sm[:, c_ssq:c_ssq + 1])
                    act(sm[:, 17:18], sm[:, c_ssq:c_ssq + 1], AF.Ln, [sr, mr], [sr], bias=EPSB[:, 1 - idx:2 - idx], scale=mulv)
                    act(sm[:, c_r:c_r + 1], sm[:, 17:18], AF.Exp, [sr], [sr], scale=-0.5)
                    n_tok = B128.next()
                    ts(DVE, n_tok[:, :], pa, sm[:, c_r:c_r + 1], None, ALU.mult, None, [ra, sr], [n_tok.r()])
                    cb2 = 192 + 64 * idx
                    rb = PR(bQ, cb2, cb2 + 64)
                    pb2 = psb(bQ, cb2, cb2 + 64)
                    tr(pb2, n_tok[:, :], identB[:, :], [n_tok.r(), identB.r()], [rb])
                    nT = B128.next()
                    cp(ACT, nT[:, :], pb2, [rb], [nT.r()])
                    res.append((n_tok, nT))
                (qn, qnT), (kn, knT) = res
                rv = PR(bQ, 320, 448)
                for c2 in range(2):
                    tr(psb(bQ, 320 + 64 * c2, 384 + 64 * c2), pT[2 + c2][:, tsl], identB[:, :], [pT[2 + c2].r(), identB.r()], [rv])
                vb = B256.next()
                ts(DVE, vb[:, :], psb(bQ, 320, 448), sm[:, 0:1], None, ALU.mult, None, [rv, sr], [vb.r()])
                gbc = F128.next()
                ts(DVE, gbc[:, :], C("ones"), sm[:, 3:4], None, ALU.mult, None, [cr, sr], [gbc.r()])
                gU = F128.next()
                ts(DVE, gU[:, :], C("Uc"), sm[:, 3:4], None, ALU.mult, None, [cr, sr], [gU.r()])
                rD, rDT, rG = PR(bR, 0, 128), PR(bR, 128, 256), PR(bQ, 448, 512)
                mm(PSf(bR, 0, 128), gU[:, :], C("ones"), True, False, [gU.r(), cr], [rD])
                mm(PSf(bR, 0, 128), gbc[:, :], C("nUc"), False, True, [gbc.r(), cr], [rD])
                mm(PSf(bR, 128, 256), gU[:, :], C("nones"), True, False, [gU.r(), cr], [rDT])
                mm(PSf(bR, 128, 256), gbc[:, :], C("Uc"), False, True, [gbc.r(), cr], [rDT])
                mm(PSf(bQ, 448, 450), C("Uc"), sm[:, 3:5], True, True, [cr, sr], [rG])
                mm(PSf(bQ, 452, 454), C("ones"), sm[:, 3:5], True, True, [cr, sr], [rG])
                decs = []
                for (c0, nm, rr) in ((0, "negm", rD), (128, "negmT", rDT)):
                    d0 = F128.next()
                    stt(d0[:, :], PSf(bR, c0, c0 + 128), 0.0, C(nm), ALU.min, ALU.add, [rr, cr], [d0.r()])
                    d1 = F128.next() if c0 == 0 else DECT.next()
                    act(d1[:, :], d0[:, :], AF.Exp, [d0.r()], [d1.r()])
                    decs.append(d1)
                dec, decT = decs
                act(sm[:, 9:10], PSf(bQ, 448, 449), AF.Exp, [rG], [sr])
                cp(DVE, sm[:, 10:11], PSf(bQ, 452, 453), [rG], [sr])
                act(sm[:, 11:12], PSf(bQ, 452, 453), AF.Exp, [rG], [sr])
                act(sm[:, 12:13], PSf(bQ, 448, 449), AF.Exp, [rG, sr], [sr], bias=sm[:, 10:11], scale=-1.0)
                tt(DVE, sm[:, 13:14], sm[:, 0:1], sm[:, 9:10], ALU.mult, [sr], [sr])
                rKK = PR(bS, 0, 128)
                mm(PSf(bS, 0, 128), knT[:, :], knT[:, :], True, True, [knT.r()], [rKK])
                ndbd = F128.next()
                tt(POOL, ndbd[:, :], dec[:, :], C("nbd"), ALU.mult, [dec.r(), cr], [ndbd.r()])
                dlo = F128.next()
                tt(POOL, dlo[:, :], dec[:, :], C("lom"), ALU.mult, [dec.r(), cr], [dlo.r()])
                Pk = F128.next()
                stt(Pk[:, :], PSf(bS, 0, 128), sm[:, 0:1], ndbd[:, :], ALU.mult, ALU.mult, [rKK, sr, ndbd.r()], [Pk.r()])
                Lo = LO.next()
                stt(Lo[:, :], PSf(bS, 0, 128), sm[:, 0:1], dlo[:, :], ALU.mult, ALU.mult, [rKK, sr, dlo.r()], [Lo.r()])
                rb_, rc_, rd_ = PR(bS, 128, 256), PR(bS, 256, 384), PR(bS, 384, 512)
                tr(PSf(bS, 128, 256), Pk[:, :], IF, [Pk.r(), cr], [rb_])
                Mk = F128.next()
                cp(ACT, Mk[:, :], PSf(bS, 128, 256), [rb_], [Mk.r()])
                R = F128.next()
                tt(DVE, R[:, :], Mk[:, :], IF, ALU.add, [Mk.r(), cr], [R.r()])
                for k in range(5):
                    if k < 4:
                        mm(PSf(bS, 256, 384), Pk[:, :], Mk[:, :], True, True, [Pk.r(), Mk.r()], [rc_])
                    mm(PSf(bS, 384, 512), Mk[:, :], Pk[:, :], True, True, [Pk.r(), Mk.r()], [rd_])
                    Pn = F128.next()
                    cp(ACT, Pn[:, :], PSf(bS, 384, 512), [rd_], [Pn.r()])
                    if k < 4:
                        Mn = F128.next()
                        cp(DVE, Mn[:, :], PSf(bS, 256, 384), [rc_], [Mn.r()])
                    mm(PSf(bS, 0, 128), Pn[:, :], R[:, :], True, True, [Pn.r(), R.r()], [rKK])
                    Rn = F128.next()
                    tt(DVE, Rn[:, :], R[:, :], PSf(bS, 0, 128), ALU.add, [R.r(), rKK], [Rn.r()])
                    R = Rn
                    Pk = Pn
                    if k < 4:
                        Mk = Mn
                TdT = R
                tr(PSf(bS, 128, 256), TdT[:, :], IF, [TdT.r(), cr], [rb_])
                Td = F128.next()
                cp(ACT, Td[:, :], PSf(bS, 128, 256), [rb_], [Td.r()])
                mm(PSf(bS, 256, 384), Lo[:, :], TdT[:, :], True, True, [Lo.r(), TdT.r()], [rc_])
                X = F128.next()
                cp(DVE, X[:, :], PSf(bS, 256, 384), [rc_], [X.r()])
                mm(PSf(bS, 384, 512), Td[:, :], X[:, :], True, True, [Td.r(), X.r()], [rd_])
                TT_ = B128.next()
                tt(DVE, TT_[:, :], TdT[:, :], PSf(bS, 384, 512), ALU.subtract, [TdT.r(), rd_], [TT_.r()])
                ru, rws = PR(bP, 0, 256), PR(bP, 256, 512)
                mm(PSf(bP, 0, 256), TT_[:, :], vb[:, :], True, True, [TT_.r(), vb.r()], [ru])
                u = F256.next()
                cp(ACT, u[:, :], PSf(bP, 0, 256), [ru], [u.r()])
                kbg = B128.next()
                ts(DVE, kbg[:, :], kn[:, :], sm[:, 13:14], None, ALU.mult, None, [kn.r(), sr], [kbg.r()])
                rw_, rqk = PR(bR, 256, 384), PR(bR, 384, 512)
                mm(PSf(bR, 256, 384), kbg[:, :], TT_[:, :], True, True, [kbg.r(), TT_.r()], [rw_])
                wT = B128.next()
                cp(ACT, wT[:, :], PSf(bR, 256, 384), [rw_], [wT.r()])
                kg = B128.next()
                ts(DVE, kg[:, :], kn[:, :], sm[:, 12:13], None, ALU.mult, None, [kn.r(), sr], [kg.r()])
                mm(PSf(bR, 384, 512), knT[:, :], qnT[:, :], True, True, [knT.r(), qnT.r()], [rqk])
                AT = B128.next()
                tt(DVE, AT[:, :], PSf(bR, 384, 512), decT[:, :], ALU.mult, [rqk, decT.r()], [AT.r()])
                while chain_done[0] < tl:
                    coop.yield_()
                mm(PSf(bP, 256, 512), wT[:, :], Sb[:, :], True, True, [wT.r(), Sb.r()], [rws])
                vnew = B256.next()
                tt(DVE, vnew[:, :], u[:, :], PSf(bP, 256, 512), ALU.subtract, [u.r(), rws], [vnew.r()])
                rqs, roi, rkv = PR(bQ, 0, 256), PR(bQ, 256, 512), PR(bR, 0, 256)
                mm(PSf(bQ, 0, 256), qnT[:, :], Sb[:, :], True, True, [qnT.r(), Sb.r()], [rqs])
                t1 = F256.next()
                act(t1[:, :], PSf(bQ, 0, 256), AF.Identity, [rqs, sr], [t1.r()], scale=sm[:, 9:10])
                mm(PSf(bQ, 256, 512), AT[:, :], vnew[:, :], True, True, [AT.r(), vnew.r()], [roi])
                o = F256.next()
                tt(DVE, o[:, :], PSf(bQ, 256, 512), t1[:, :], ALU.add, [roi, t1.r()], [o.r()])
                mm(PSf(bR, 0, 256), kg[:, :], vnew[:, :], True, True, [kg.r(), vnew.r()], [rkv])
                stt(Sf, Sf, sm[:, 11:12], PSf(bR, 0, 256), ALU.mult, ALU.add, [sfr, sr, rkv], [sfr])
                cp(ACT, Sb[:, :], Sf, [sfr], [Sb.r()])
                chain_done[0] = tl + 1
                junk2 = F256.next()
                act(junk2[:, :], o[:, :], AF.Square, [o.r()], [junk2.r(), sr], accum=sm[:, 14:15])
                ts(DVE, sm[:, 15:16], sm[:, 14:15], 1.0 / 256.0, 1e-6, ALU.mult, ALU.add, [sr], [sr])
                pw(sm[:, 16:17], sm[:, 15:16], nhalf[:, 0:1], [sr, nhalf.r()], [sr])
                y = B256.next()
                stt(y[:, :], o[:, :], sm[:, 16:17], gate[:, :], ALU.mult, ALU.mult, [o.r(), sr, gate.r()], [y.r()])
                y_transposes(y, yT, tsl, bS)

            coop.run([(lambda tl=tl: tile_body(tl)) for tl in range(4)], depth=GDN_DEPTH)
            out_proj(slot, yT, blk, gm)

    def phase_final(row0):
        for blk in range(NBLK):
            rs = rstd_block(blk)
            for dc in range(8):
                o = F512.next()
                stt(o[:, :], xT[:, dc, BS(blk)], Pm("now", dc, dc + 1), rs[:, :], ALU.mult, ALU.mult,
                    [xr(dc, blk), pr_, rs.r()], [o.r()])
                bank = dc % 4
                for tl in range(4):
                    tr(PSf(bank, tl * 128, (tl + 1) * 128), o[:, tl * 128:(tl + 1) * 128], C("identF"), [o.r(), cr], [PR(bank)])
                ob = OB.next()
                q = ACT if dc % 2 == 0 else DVE
                cp(q, ob[:, :], PSf(bank), [PR(bank)], [ob.r()])
                r0 = row0 + blk * 512
                dma(SP, y_d[r0:r0 + 512, dc * 128:(dc + 1) * 128].rearrange("(t p) c -> p t c", p=128),
                    ob[:, :].rearrange("p (t c) -> p t c", t=4), [ob.r()], [], ob.name)

    for pi in range(n_pass):
        s, hf = pi // 2, pi % 2
        row0 = s * SEQ + hf * TOK
        pe_barrier()
        phase_load(row0)
        phase_norm_mix(s)
        jobs = []
        if "mix_ret" in stages:
            jobs += list(range(4))
        if "mix_gdn" in stages:
            jobs += list(range(4, 8))
        if jobs:
            full_barrier()
            nxt = load_job(jobs[0])
            for ji, job in enumerate(jobs):
                slot = nxt
                if ji + 1 < len(jobs):
                    nxt = load_job(jobs[ji + 1])
                pe_barrier()
                if job < 4:
                    ret_job(s, hf, job, slot)
                else:
                    gdn_job(s, hf, job - 4, slot)
        if jobs:
            full_barrier()
        if "moe" in stages:
            pe_barrier()
            phase_norm_ffn(s)
            pe_barrier()
            phase_moe(s)
        pe_barrier()
        phase_final(row0)
    for t_ in OB.tiles:
        od = dsem(t_.name)
        nc.sync.wait_ge(od.sem, od.n)
    return nc


def _host_pack(inp):
    f = lambda a: np.ascontiguousarray(np.asarray(a, dtype=np.float32))
    w_in = f(inp["w_in"])[0]
    w_out = f(inp["w_out"])[0]
    wj = np.zeros((8, DM, WCOLS), np.float32)
    wo = np.zeros((8, 256, DM), np.float32)
    for h in range(4):
        q = w_in[:, O_RQ + h * 128:O_RQ + (h + 1) * 128]
        k = w_in[:, O_RK + h * 128:O_RK + (h + 1) * 128]
        sw = lambda a: np.concatenate([a[:, 64:128], a[:, 0:64]], axis=1)
        wj[h, :, 0:128] = q
        wj[h, :, 128:256] = sw(q)
        wj[h, :, 256:384] = k
        wj[h, :, 384:512] = sw(k)
        wj[h, :, 512:768] = w_in[:, O_RV + h * 256:O_RV + (h + 1) * 256]
        wj[h, :, 768:1024] = w_in[:, O_RG + h * 256:O_RG + (h + 1) * 256]
        wj[h, :, 1024:1280] = w_in[:, O_MA + h * 256:O_MA + (h + 1) * 256]
        g = 4 + h
        wj[g, :, 0:128] = w_in[:, O_GQ + h * 128:O_GQ + (h + 1) * 128]
        wj[g, :, 128:256] = w_in[:, O_GK + h * 128:O_GK + (h + 1) * 128]
        wj[g, :, 256:512] = w_in[:, O_GV + h * 256:O_GV + (h + 1) * 256]
        wj[g, :, 512:768] = w_in[:, O_GZ + h * 256:O_GZ + (h + 1) * 256]
        wj[g, :, 768:1024] = w_in[:, O_MB + h * 256:O_MB + (h + 1) * 256]
        wj[g, :, 1024] = w_in[:, O_GA + h]
        wj[g, :, 1025] = w_in[:, O_GB + h]
        wo[h] = w_out[h * 256:(h + 1) * 256, :]
        wo[g] = w_out[h * 256:(h + 1) * 256, :]
    prm = np.zeros((128, PRM_N), np.float32)

    def put(name, arr):
        o, w = PRM_OFF[name]
        prm[:, o:o + w] = arr

    fm = lambda v: f(v).reshape(-1, 128).T
    put("nmw", fm(inp["norm_mix_w"][0]))
    put("nfw", fm(inp["norm_ffn_w"][0]))
    put("now", fm(inp["norm_out_w"]))
    put("modb", fm(inp["mod_b"][0]))
    cw = f(inp["gdn_conv_w"])[0]
    put("convw", cw.reshape(4, 16, 128).transpose(2, 1, 0).reshape(128, 64))
    put("gnw", np.broadcast_to(f(inp["gdn_norm_w"])[0][None, :], (128, 256)))
    put("alog", np.broadcast_to(f(inp["gdn_a_log"])[0][None, :], (128, 4)))
    put("dtb", np.broadcast_to(f(inp["gdn_dt_bias"])[0][None, :], (128, 4)))
    rb = np.concatenate([f(inp["b_group"])[0].reshape(-1), f(inp["b_router"])[0].reshape(-1)])
    put("rbias", np.broadcast_to(rb[None, :], (128, 20)))
    wr = np.concatenate([f(inp["w_group"])[0], f(inp["w_router"])[0]], axis=1)
    put("Wrt", wr.reshape(8, 128, 20).transpose(1, 0, 2).reshape(128, 160))
    cst_np, rope_np = make_consts()
    shared = {
        "cst": cst_np, "rope": rope_np,
        "modw": f(inp["mod_w"])[0],
        "wj": wj, "wo": wo,
        "wg": f(inp["w_gate"])[0].reshape(16, DM, 512),
        "wu": f(inp["w_up"])[0].reshape(16, DM, 512),
        "wd": f(inp["w_down"])[0].reshape(16, 512, DM),
    }
    x = f(inp["x"])
    c = f(inp["c"])
    in_maps = []
    for core in range(NCORES):
        p = prm.copy()
        o, w = PRM_OFF["cT"]
        cc = c[core * SPC:(core + 1) * SPC]
        p[:, o:o + w] = cc.T.reshape(8, 128, 2).transpose(1, 0, 2).reshape(128, 16)
        m = dict(shared)
        m["prm"] = p
        m["x"] = x[core * SPC:(core + 1) * SPC].reshape(SPC * SEQ, DM)
        in_maps.append(m)
    return in_maps


_NC_CACHE = {}


def kernel(**inputs):
    in_maps = _host_pack(inputs)
    if "nc" not in _NC_CACHE:
        _NC_CACHE["nc"] = build_program()
    nc = _NC_CACHE["nc"]
    res = run_bass_kernel_spmd(nc, in_maps, core_ids=list(range(NCORES)))
    out = np.stack([r["y"].reshape(SPC, SEQ, DM) for r in res.results], 0).reshape(NCORES * SPC, SEQ, DM)
    return np.ascontiguousarray(out.astype(np.float32))
```
